# Optimizing a Trainium2 kernel written in Bass

```python
import jax, jax.numpy as jnp
from jax import lax
import numpy as np

D_MODEL = 4096
BATCH = 2
SEQ = 4096
DEPTH = 2

CHUNK = 64
D_MIX = D_MODEL
HEAD_DIM = 128
CONV_CH = D_MIX // 4
N_FOX_HEADS = (D_MIX - CONV_CH) // (2 * HEAD_DIM)
N_DN_HEADS = N_FOX_HEADS
FOX_W = N_FOX_HEADS * HEAD_DIM
DN_W = N_DN_HEADS * HEAD_DIM
CONF_KERNEL = 31
SHORT_CONV = 4
Q_BLOCK = 128
N_EXPERTS = 16
N_EXPERT_GROUPS = 4
EXPERTS_PER_GROUP = N_EXPERTS // N_EXPERT_GROUPS
TOP_K = 2
D_EXPERT = D_MODEL // 4
EXPERT_BLOCK = 128
EPS = 1e-6
IN_SPLITS = (CONV_CH, CONV_CH,
             FOX_W, FOX_W, FOX_W, N_FOX_HEADS, FOX_W,
             DN_W, DN_W, DN_W, N_DN_HEADS, N_DN_HEADS, DN_W)
N_IN = sum(IN_SPLITS)

kernel_name = 'hybrid_conv_fox_gdn_grouped_moe'


def rms_norm(x, g):
    xf = x.astype(jnp.float32)
    y = xf * lax.rsqrt(jnp.mean(xf * xf, axis=-1, keepdims=True) + EPS)
    return (y * g.astype(jnp.float32)).astype(x.dtype)


def layer_norm(x, g, b):
    xf = x.astype(jnp.float32)
    mu = jnp.mean(xf, axis=-1, keepdims=True)
    var = jnp.mean(jnp.square(xf - mu), axis=-1, keepdims=True)
    y = (xf - mu) * lax.rsqrt(var + EPS)
    return (y * g.astype(jnp.float32) + b.astype(jnp.float32)).astype(x.dtype)


def l2_norm(x):
    return x * lax.rsqrt(jnp.sum(x * x, axis=-1, keepdims=True) + EPS)


def causal_depthwise_conv(x, w):
    k_w, ch = w.shape
    xp = jnp.pad(x, ((0, 0), (k_w - 1, 0), (0, 0)))
    return lax.conv_general_dilated(xp, w[:, None, :].astype(x.dtype), window_strides=(1,),
                                    padding='VALID', dimension_numbers=('NWC', 'WIO', 'NWC'),
                                    feature_group_count=ch)


def conformer_conv(a_val, a_gate, conv_w, conv_b, ln_g, ln_b):
    a = a_val * jax.nn.sigmoid(a_gate)
    a = causal_depthwise_conv(a, conv_w) + conv_b
    a = layer_norm(a, ln_g, ln_b)
    return jax.nn.silu(a)


def forgetting_attention(q, k, v, f_logit, f_bias, qn_g, kn_g):
    _, seq, _, dh = q.shape
    q = rms_norm(q, qn_g).transpose(0, 2, 1, 3)
    k = rms_norm(k, kn_g).transpose(0, 2, 1, 3)
    v = v.transpose(0, 2, 1, 3)
    log_f = jax.nn.log_sigmoid(f_logit.astype(jnp.float32) + f_bias.astype(jnp.float32))
    cum = jnp.cumsum(log_f, axis=1).transpose(0, 2, 1)
    scale = dh ** -0.5
    outs = []
    for blk in range(seq // Q_BLOCK):
        q0, q1 = blk * Q_BLOCK, (blk + 1) * Q_BLOCK
        s = jnp.einsum('bhqd,bhkd->bhqk', q[:, :, q0:q1], k[:, :, :q1],
                       preferred_element_type=jnp.float32) * scale
        s = s + cum[:, :, q0:q1, None] - cum[:, :, None, :q1]
        qpos = jnp.arange(q0, q1)[:, None]
        kpos = jnp.arange(q1)[None, :]
        p = jax.nn.softmax(jnp.where(kpos <= qpos, s, -jnp.inf), axis=-1)
        outs.append(jnp.einsum('bhqk,bhkd->bhqd', p.astype(v.dtype), v[:, :, :q1]))
    return jnp.concatenate(outs, axis=2).transpose(0, 2, 1, 3)


def gated_delta_rule(q, k, v, g, beta):
    bsz, seq, nh, dk = q.shape
    dv = v.shape[-1]
    n_ch = seq // CHUNK
    f32 = jnp.float32
    q = l2_norm(q.astype(f32)) * (dk ** -0.5)
    k = l2_norm(k.astype(f32))

    def chunks(t):
        return t.astype(f32).reshape(bsz, n_ch, CHUNK, nh, -1).transpose(0, 3, 1, 2, 4)

    qc, kc, vc = chunks(q), chunks(k), chunks(v)
    gc = chunks(g[..., None])[..., 0]
    bc = chunks(beta[..., None])[..., 0]
    g_cum = jnp.cumsum(gc, axis=-1)
    idx = jnp.arange(CHUNK)
    causal = idx[:, None] >= idx[None, :]
    strict = idx[:, None] > idx[None, :]
    gamma = jnp.exp(jnp.where(causal, g_cum[..., :, None] - g_cum[..., None, :], -jnp.inf))
    kb = kc * bc[..., None]
    a_mat = jnp.where(strict, jnp.einsum('bhnid,bhnjd->bhnij', kb, kc) * gamma, 0.0)
    t_mat = a_mat + jnp.eye(CHUNK, dtype=f32)
    rhs = jnp.concatenate([vc * bc[..., None], kb * jnp.exp(g_cum)[..., None]], axis=-1)
    sol = lax.linalg.triangular_solve(t_mat, rhs, left_side=True, lower=True, unit_diagonal=True)
    u, w = sol[..., :dv], sol[..., dv:]
    qk = jnp.where(causal, jnp.einsum('bhnid,bhnjd->bhnij', qc, kc) * gamma, 0.0)
    g_last = g_cum[..., -1]
    q_dec = qc * jnp.exp(g_cum)[..., None]
    k_dec = kc * jnp.exp(g_last[..., None] - g_cum)[..., None]

    def step(state, xs):
        u_n, w_n, qk_n, q_n, k_n, gl_n = xs
        v_new = u_n - jnp.einsum('bhck,bhkv->bhcv', w_n, state)
        o_n = jnp.einsum('bhck,bhkv->bhcv', q_n, state) + jnp.einsum('bhij,bhjv->bhiv', qk_n, v_new)
        state = state * jnp.exp(gl_n)[..., None, None] + jnp.einsum('bhck,bhcv->bhkv', k_n, v_new)
        return state, o_n

    xs = tuple(jnp.moveaxis(t, 2, 0) for t in (u, w, qk, q_dec, k_dec, g_last))
    state0 = jnp.zeros((bsz, nh, dk, dv), f32)
    _, o = lax.scan(step, state0, xs)
    return o.transpose(1, 0, 3, 2, 4).reshape(bsz, seq, nh, dv).astype(v.dtype)


def token_mixer(h, w_in, conv_w, conv_b, conv_ln_g, conv_ln_b, fox_f_bias, fox_qn_g, fox_kn_g,
                fox_on_g, dn_conv_w, dn_a_log, dn_dt_bias, dn_on_g, w_out):
    bsz, seq, _ = h.shape
    proj = jnp.einsum('bsd,dn->bsn', h, w_in)
    cuts = [int(i) for i in np.cumsum(IN_SPLITS)[:-1]]
    (a_val, a_gate, fq, fk, fv, ff, fg, dq, dk, dv, db, da, dz) = jnp.split(proj, cuts, axis=-1)

    def heads(t):
        return t.reshape(bsz, seq, -1, HEAD_DIM)

    y_a = conformer_conv(a_val, a_gate, conv_w, conv_b, conv_ln_g, conv_ln_b)
    o_b = forgetting_attention(heads(fq), heads(fk), heads(fv), ff, fox_f_bias, fox_qn_g, fox_kn_g)
    y_b = (rms_norm(o_b, fox_on_g) * jax.nn.sigmoid(heads(fg))).reshape(bsz, seq, FOX_W)
    qkv = jax.nn.silu(causal_depthwise_conv(jnp.concatenate([dq, dk, dv], axis=-1), dn_conv_w))
    cq, ck, cv = jnp.split(qkv, [DN_W, 2 * DN_W], axis=-1)
    beta = jax.nn.sigmoid(db.astype(jnp.float32))
    g = -jnp.exp(dn_a_log.astype(jnp.float32)) * jax.nn.softplus(
        da.astype(jnp.float32) + dn_dt_bias.astype(jnp.float32))
    o_c = gated_delta_rule(heads(cq), heads(ck), heads(cv), g, beta)
    y_c = (rms_norm(o_c, dn_on_g) * jax.nn.silu(heads(dz))).reshape(bsz, seq, DN_W)
    y = jnp.concatenate([y_a, y_b, y_c], axis=-1)
    return jnp.einsum('bsm,md->bsd', y, w_out)


def moe_ffn(h, w_router, router_bias, w_gate, w_up, w_down):
    bsz, seq, d = h.shape
    n_tok = bsz * seq
    ht = h.reshape(n_tok, d)
    scores = jax.nn.sigmoid(jnp.einsum('td,de->te', ht, w_router, preferred_element_type=jnp.float32))
    sel = (scores + router_bias.astype(jnp.float32)).reshape(n_tok, N_EXPERT_GROUPS, EXPERTS_PER_GROUP)
    group_score = jnp.sum(lax.top_k(sel, 2)[0], axis=-1)
    best_group = jnp.argmax(group_score, axis=-1)
    in_group = jnp.take_along_axis(sel, best_group[:, None, None], axis=1)[:, 0]
    _, local = lax.top_k(in_group, TOP_K)
    expert_idx = best_group[:, None] * EXPERTS_PER_GROUP + local
    gate = jnp.take_along_axis(scores, expert_idx, axis=-1)
    gate = gate / jnp.sum(gate, axis=-1, keepdims=True)

    n_asg = n_tok * TOP_K
    flat_e = expert_idx.reshape(n_asg)
    order = jnp.argsort(flat_e)
    sorted_e = flat_e[order]
    counts = jnp.bincount(flat_e, length=N_EXPERTS)
    padded = (counts + EXPERT_BLOCK - 1) // EXPERT_BLOCK * EXPERT_BLOCK
    seg_start = jnp.cumsum(counts) - counts
    pad_end = jnp.cumsum(padded)
    pad_start = pad_end - padded
    dest_sorted = (pad_start[sorted_e] + jnp.arange(n_asg) - seg_start[sorted_e]).astype(jnp.int32)
    n_rows = n_asg + N_EXPERTS * EXPERT_BLOCK
    n_blocks = n_rows // EXPERT_BLOCK
    row_token = jnp.zeros((n_rows,), jnp.int32).at[dest_sorted].set((order // TOP_K).astype(jnp.int32))
    block_expert = jnp.minimum(
        jnp.searchsorted(pad_end, jnp.arange(n_blocks) * EXPERT_BLOCK, side='right'), N_EXPERTS - 1)
    rows = ht[row_token].reshape(n_blocks, EXPERT_BLOCK, d)

    def expert_block(args):
        xb, e = args
        return (jax.nn.silu(xb @ w_gate[e]) * (xb @ w_up[e])) @ w_down[e]

    out_rows = lax.map(expert_block, (rows, block_expert)).reshape(n_rows, d)
    dest = jnp.zeros((n_asg,), jnp.int32).at[order].set(dest_sorted)
    y = out_rows[dest].reshape(n_tok, TOP_K, d)
    y = jnp.einsum('tkd,tk->td', y, gate.astype(y.dtype))
    return y.reshape(bsz, seq, d)


def setup_inputs(seed: int = 0) -> dict:
    key = jax.random.key(seed)
    ks = jax.random.split(key, 28)
    f32 = jnp.float32
    L, D = DEPTH, D_MODEL

    def nrm(k, shape, scale):
        return jax.random.normal(k, shape, f32) * scale

    dt = jnp.exp(jax.random.uniform(ks[17], (L, N_DN_HEADS), f32, np.log(1e-3), np.log(1e-1)))
    return {
        'x': nrm(ks[0], (BATCH, SEQ, D), 1.0),
        'c': nrm(ks[1], (BATCH, D), 1.0),
        'ada_w': nrm(ks[2], (L, D, 6 * D), 0.5 * D ** -0.5),
        'ada_b': nrm(ks[3], (L, 6 * D), 0.02),
        'norm_mix_g': 1.0 + nrm(ks[4], (L, D), 0.02),
        'norm_ffn_g': 1.0 + nrm(ks[5], (L, D), 0.02),
        'w_in': nrm(ks[6], (L, D, N_IN), D ** -0.5),
        'conv_w': nrm(ks[7], (L, CONF_KERNEL, CONV_CH), CONF_KERNEL ** -0.5),
        'conv_b': nrm(ks[8], (L, CONV_CH), 0.02),
        'conv_ln_g': 1.0 + nrm(ks[9], (L, CONV_CH), 0.02),
        'conv_ln_b': nrm(ks[10], (L, CONV_CH), 0.02),
        'fox_f_bias': jax.random.uniform(ks[11], (L, N_FOX_HEADS), f32, 2.0, 5.0),
        'fox_qn_g': 1.0 + nrm(ks[12], (L, HEAD_DIM), 0.02),
        'fox_kn_g': 1.0 + nrm(ks[13], (L, HEAD_DIM), 0.02),
        'fox_on_g': 1.0 + nrm(ks[14], (L, HEAD_DIM), 0.02),
        'dn_conv_w': nrm(ks[15], (L, SHORT_CONV, 3 * DN_W), SHORT_CONV ** -0.5),
        'dn_a_log': jnp.log(jax.random.uniform(ks[16], (L, N_DN_HEADS), f32, 1.0, 16.0)),
        'dn_dt_bias': dt + jnp.log(-jnp.expm1(-dt)),
        'dn_on_g': 1.0 + nrm(ks[18], (L, HEAD_DIM), 0.02),
        'w_out': nrm(ks[19], (L, D_MIX, D), D_MIX ** -0.5),
        'w_router': nrm(ks[20], (D, N_EXPERTS), D ** -0.5),
        'router_bias': nrm(ks[21], (N_EXPERTS,), 0.01),
        'w_gate_e': nrm(ks[22], (L, N_EXPERTS, D, D_EXPERT), D ** -0.5),
        'w_up_e': nrm(ks[23], (L, N_EXPERTS, D, D_EXPERT), D ** -0.5),
        'w_down_e': nrm(ks[24], (L, N_EXPERTS, D_EXPERT, D), D_EXPERT ** -0.5),
        'final_g': 1.0 + nrm(ks[25], (D,), 0.02),
    }


def reference(x, c, ada_w, ada_b, norm_mix_g, norm_ffn_g, w_in, conv_w, conv_b, conv_ln_g,
              conv_ln_b, fox_f_bias, fox_qn_g, fox_kn_g, fox_on_g, dn_conv_w, dn_a_log, dn_dt_bias,
              dn_on_g, w_out, w_router, router_bias, w_gate_e, w_up_e, w_down_e, final_g):
    c_act = jax.nn.silu(c)
    for l in range(DEPTH):
        mod = (jnp.einsum('bd,de->be', c_act, ada_w[l]) + ada_b[l])[:, None, :]
        shift_m, scale_m, gate_m, shift_f, scale_f, gate_f = jnp.split(mod, 6, axis=-1)
        h = rms_norm(x, norm_mix_g[l]) * (1.0 + scale_m) + shift_m
        y = token_mixer(h, w_in[l], conv_w[l], conv_b[l], conv_ln_g[l], conv_ln_b[l], fox_f_bias[l],
                        fox_qn_g[l], fox_kn_g[l], fox_on_g[l], dn_conv_w[l], dn_a_log[l],
                        dn_dt_bias[l], dn_on_g[l], w_out[l])
        x = x + gate_m * y
        h = rms_norm(x, norm_ffn_g[l]) * (1.0 + scale_f) + shift_f
        y = moe_ffn(h, w_router, router_bias, w_gate_e[l], w_up_e[l], w_down_e[l])
        x = x + gate_f * y
    return rms_norm(x, final_g)
```

```python
import contextlib
import numpy as np
import ml_dtypes
import concourse.bass as bass
import concourse.mybir as mybir
from concourse.bass_utils import run_bass_kernel_spmd

F32 = mybir.dt.float32
BF16 = mybir.dt.bfloat16
AF = mybir.ActivationFunctionType
ALU = mybir.AluOpType
AX = mybir.AxisListType

D = 4096
SEQ = 4096
NB = 2
DEPTH = 2
HD = 128
CONV_CH = 1024
NH = 12
FOX_W = 1536
DN_W = 1536
N_IN = 14372
NE = 16
DE = 1024
EPS = 1e-6
KC = D // 128

O_AVAL = 0
O_AGATE = 1024
O_FQ = 2048
O_FK = O_FQ + FOX_W
O_FV = O_FK + FOX_W
O_FF = O_FV + FOX_W
O_FG = O_FF + NH
O_DQ = O_FG + FOX_W
O_DK = O_DQ + DN_W
O_DV = O_DK + DN_W
O_DB = O_DV + DN_W
O_DA = O_DB + NH
O_DZ = O_DA + NH
assert O_DZ + DN_W == N_IN


class Trk:
    __slots__ = ("w", "r", "sem", "semcnt", "name", "excl")

    def __init__(self, name=""):
        self.excl = False
        self.w = {}
        self.r = {}
        self.sem = None
        self.semcnt = 0
        self.name = name


class Tile:
    def __init__(self, t, name):
        self.t = t
        self.trk = Trk(name)
        self.subs = {}

    def __getitem__(self, idx):
        return self.t[idx]

    def sub(self, key):
        if key not in self.subs:
            self.subs[key] = Trk(f"{self.trk.name}.{key}")
        return self.subs[key]


def _trk(x):
    return x.trk if isinstance(x, Tile) else x


import os as _os
SAME_ENGINE_SYNC = _os.environ.get("K_SES", "1") == "1"


class Prog:
    ENGS = ("tensor", "vector", "scalar", "gpsimd", "sync")

    def __init__(self, same_engine_sync=SAME_ENGINE_SYNC):
        self.nc = bass.Bass("TRN2", target_bir_lowering=False)
        self.stack = contextlib.ExitStack()
        self.same_engine_sync = same_engine_sync
        self.esem = {}
        self.ecnt = {}
        self.known = {e: {} for e in self.ENGS}
        self.ops = {e: [] for e in self.ENGS}
        self.sems = {}
        for e in ("tensor", "vector", "scalar", "gpsimd"):
            s = self.stack.enter_context(self.nc.semaphore(f"e_{e}"))
            self.esem[e] = s
            self.sems[id(s)] = s
            self.ecnt[e] = 0
        self.free_dsems = []
        self.ndsem = 0
        self.phase_stack = None
        self.uid = 0
        self.all_dma_trk = []
        self.phase_trks = []
        self.dsem_latest = {}

    def dram(self, name, shape, dt, kind, **kw):
        t = self.nc.dram_tensor(name, list(shape), dt, kind=kind, **kw).ap()
        return Tile(t, name)

    def begin_phase(self):
        self.phase_stack = contextlib.ExitStack()

    def sb(self, shape, dt, name=None):
        self.uid += 1
        name = name or f"t{self.uid}"
        t = self.phase_stack.enter_context(self.nc.sbuf_tensor(f"{name}_{self.uid}", list(shape), dt))
        return Tile(t, name)

    def ps(self, shape, dt=F32, name=None):
        self.uid += 1
        name = name or f"p{self.uid}"
        t = self.phase_stack.enter_context(self.nc.psum_tensor(f"{name}_{self.uid}", list(shape), dt))
        tl = Tile(t, name)
        tl.trk.excl = True
        return tl

    def _dsem(self, trk):
        if trk.sem is None:
            if self.free_dsems:
                s = self.free_dsems.pop()
                trk.semcnt = self.dsem_latest.get(id(s), 0)
            else:
                self.ndsem += 1
                s = self.stack.enter_context(self.nc.semaphore(f"d{self.ndsem}"))
                self.sems[id(s)] = s
            trk.sem = s
            self.phase_trks.append(trk)
        return trk.sem

    def _collect(self, eng, reads, writes):
        need = {}
        for b in reads:
            for k, v in _trk(b).w.items():
                if need.get(k, 0) < v:
                    need[k] = v
        for b in writes:
            tb = _trk(b)
            for dct in (tb.w, tb.r):
                for k, v in dct.items():
                    if need.get(k, 0) < v:
                        need[k] = v
        waits = []
        kn = self.known[eng]
        own = id(self.esem[eng]) if eng in self.esem else None
        for k, v in need.items():
            if k == own and (eng == "tensor" or not self.same_engine_sync):
                continue
            if kn.get(k, 0) >= v:
                continue
            kn[k] = v
            waits.append((self.sems[k], v))
        return waits

    def _mark(self, ev, reads, writes):
        k, v = ev
        for b in reads:
            tb = _trk(b)
            if tb.r.get(k, 0) < v:
                tb.r[k] = v
        for b in writes:
            tb = _trk(b)
            tb.w = {k: v}
            tb.r = {}

    def op(self, eng, fn, reads=(), writes=()):
        ex = [b for b in reads if _trk(b).excl]
        if ex:
            reads = [b for b in reads if not _trk(b).excl]
            writes = list(writes) + ex
        waits = self._collect(eng, reads, writes)
        self.ecnt[eng] += 1
        sem = self.esem[eng]
        self.ops[eng].append((waits, fn, sem, 1))
        self._mark((id(sem), self.ecnt[eng]), reads, writes)

    def dma(self, out_ap, in_ap, reads=(), writes=(), semtrk=None, q="sync", **kw):
        self.dma_group([(out_ap, in_ap)], reads, writes, semtrk, q, **kw)

    def dma_group(self, pairs, reads=(), writes=(), semtrk=None, q="sync", **kw):
        semtrk = _trk(semtrk)
        sem = self._dsem(semtrk)
        waits = self._collect(q, reads, writes)
        kn = self.known[q]
        if semtrk.semcnt and kn.get(id(sem), 0) < semtrk.semcnt:
            kn[id(sem)] = semtrk.semcnt
            waits = [w for w in waits if w[0] is not sem] + [(sem, semtrk.semcnt)]
        for i, (out_ap, in_ap) in enumerate(pairs):
            semtrk.semcnt += 16

            def fn(e, out_ap=out_ap, in_ap=in_ap, kw=kw):
                return e.dma_start(out=out_ap, in_=in_ap, **kw)

            self.ops[q].append((waits if i == 0 else [], fn, sem, 16))
        self.dsem_latest[id(sem)] = semtrk.semcnt
        self._mark((id(sem), semtrk.semcnt), reads, writes)

    def coll(self, kind, out_tile, in_tile, groups, op=None, inc=1):
        ctrk = Trk("coll")
        sem = self._dsem(ctrk)
        waits = self._collect("gpsimd", [in_tile], [out_tile])
        kn = self.known["gpsimd"]
        if ctrk.semcnt and kn.get(id(sem), 0) < ctrk.semcnt:
            kn[id(sem)] = ctrk.semcnt
            waits = [w for w in waits if w[0] is not sem] + [(sem, ctrk.semcnt)]
        ctrk.semcnt += inc
        op = op if op is not None else ALU.bypass

        def fn(e):
            return e.collective_compute(kind, op, groups, [in_tile.t.opt()], [out_tile.t.opt()])

        self.ops["gpsimd"].append((waits, fn, sem, inc))
        self.dsem_latest[id(sem)] = ctrk.semcnt
        self._mark((id(sem), ctrk.semcnt), [in_tile], [out_tile])

    def barrier_all(self):
        tgt = {}
        for e, s in self.esem.items():
            if self.ecnt[e]:
                tgt[id(s)] = self.ecnt[e]
        for k, v in self.dsem_latest.items():
            if v:
                tgt[k] = v
        for e in self.ENGS:
            waits = []
            kn = self.known[e]
            for k, v in tgt.items():
                if kn.get(k, 0) < v:
                    kn[k] = v
                    waits.append((self.sems[k], v))
            if waits:
                self.ops[e].append((waits, None, None, 0))

    def end_phase(self):
        self.barrier_all()
        nc = self.nc
        ops = self.ops
        with nc.Block() as block:
            def mk(ename):
                def body(e):
                    for waits, fn, sem, amt in ops[ename]:
                        for s, v in waits:
                            e.wait_ge(s, v)
                        if fn is not None:
                            fn(e).then_inc(sem, amt)
                return body
            block.tensor(mk("tensor"))
            block.vector(mk("vector"))
            block.scalar(mk("scalar"))
            block.gpsimd(mk("gpsimd"))
            block.sync(mk("sync"))
        self.ops = {e: [] for e in self.ENGS}
        self.phase_stack.close()
        self.phase_stack = None
        for t in self.phase_trks:
            self.free_dsems.append(t.sem)
            t.sem = None
        self.phase_trks = []

    def finish(self):
        self.stack.close()
        return self.nc

    def mm(self, out, lhsT, rhs, start, stop, reads, writes):
        self.op("tensor", lambda e: e.matmul(out, lhsT, rhs, start=start, stop=stop), reads, writes)

    def act(self, out, in_, func, reads, writes, bias=None, scale=None, accum_out=None, eng="scalar"):
        kw = {}
        if bias is not None:
            kw["bias"] = bias
        if scale is not None:
            kw["scale"] = scale
        if accum_out is not None:
            kw["accum_out"] = accum_out
        self.op(eng, lambda e: e.activation(out, in_, func, **kw), reads, writes)

    def ts(self, out, in0, s1, s2, op0, op1, reads, writes, eng="vector", accum_out=None):
        kw = {}
        if accum_out is not None:
            kw["accum_out"] = accum_out
        if op1 is None:
            self.op(eng, lambda e: e.tensor_scalar(out, in0, s1, None, op0, **kw), reads, writes)
        else:
            self.op(eng, lambda e: e.tensor_scalar(out, in0, s1, s2, op0, op1, **kw), reads, writes)

    def tt(self, out, in0, in1, op, reads, writes, eng="vector"):
        self.op(eng, lambda e: e.tensor_tensor(out, in0, in1, op), reads, writes)

    def stt(self, out, in0, scalar, in1, op0, op1, reads, writes, eng="vector"):
        self.op(eng, lambda e: e.scalar_tensor_tensor(out, in0, scalar, in1, op0, op1), reads, writes)

    def copy(self, out, in_, reads, writes, eng="vector"):
        if eng == "scalar":
            self.op(eng, lambda e: e.activation(out, in_, AF.Copy), reads, writes)
        else:
            self.op(eng, lambda e: e.tensor_copy(out, in_), reads, writes)

    def memset(self, ap, val, writes, eng="vector"):
        self.op(eng, lambda e: e.memset(ap, val), (), writes)


MCOLS = 6 * D // 8


def build_mod():
    P = Prog()
    nc = P.nc
    c_in = P.dram("c", [NB, D], F32, "ExternalInput")
    aw = P.dram("ada_w", [DEPTH, D, MCOLS], F32, "ExternalInput")
    ab = P.dram("ada_b", [DEPTH, MCOLS], F32, "ExternalInput")
    out = P.dram("mod", [DEPTH, NB, MCOLS], F32, "ExternalOutput")
    P.begin_phase()
    cT = P.sb([128, KC, NB], F32, "cT")
    P.dma_group([(cT[:, :, b], c_in.t[b].rearrange("(k p) -> p k", p=128)) for b in range(NB)],
                reads=[c_in], writes=[cT], semtrk=cT, allow_slow_non_contiguous=True)
    cA = P.sb([128, KC, NB], F32, "cA")
    P.act(cA[:], cT[:], AF.Silu, [cT], [cA])
    wb = [P.sb([128, KC, 512], F32, f"w{i}") for i in range(2)]
    pss = [P.ps([128, 512], F32, f"ps{i}") for i in range(2)]
    res = P.sb([NB, DEPTH, MCOLS], F32, "res")
    bias = P.sb([NB, DEPTH, MCOLS], F32, "bias")
    for b in range(NB):
        P.dma(bias[b:b + 1, :, :], ab.t.rearrange("(o l) n -> o l n", o=1), reads=[ab], writes=[bias], semtrk=bias)
    it = 0
    for l in range(DEPTH):
        for ct in range(MCOLS // 512):
            w = wb[it % 2]
            ps = pss[it % 2]
            src = aw.t[l].rearrange("(k p) n -> p k n", p=128)[:, :, ct * 512:(ct + 1) * 512]
            half = KC // 2
            P.dma_group([(w[:, :half, :], src[:, :half, :]), (w[:, half:, :], src[:, half:, :])],
                        reads=[aw], writes=[w], semtrk=w, q="sync")
            for k in range(KC):
                P.mm(ps[:NB, :], cA[:, k, :], w[:, k, :], k == 0, k == KC - 1, [cA, w], [ps])
            P.tt(res[:, l, ct * 512:(ct + 1) * 512], ps[:NB, :], bias[:, l, ct * 512:(ct + 1) * 512], ALU.add,
                 [ps, bias], [res])
            it += 1
    P.dma(out.t.rearrange("l b n -> b l n"), res[:], reads=[res], writes=[out], semtrk=res)
    P.end_phase()
    return P.finish()


def run_mod(c, ada_w, ada_b):
    nc = build_mod()
    in_maps = []
    for i in range(8):
        sl = slice(i * MCOLS, (i + 1) * MCOLS)
        in_maps.append({"c": np.ascontiguousarray(c),
                        "ada_w": np.ascontiguousarray(ada_w[:, :, sl]),
                        "ada_b": np.ascontiguousarray(ada_b[:, sl])})
    res = run_bass_kernel_spmd(nc, in_maps, core_ids=list(range(8)))
    return np.concatenate([r["mod"] for r in res.results], axis=2)


TPC = 1024
NTT = TPC // 128


def build_B(n_exp=NE, ctx=None):
    if ctx is not None:
        P = ctx["P"]
        nc = P.nc
        (x, yT, wout, modv, gffn, fing, wr, rb, wg, wu, wd, ident_d, out, outn, x1, acc, hTd, gates_d) = (
            ctx[k] for k in ("x", "yT", "wout", "modv", "gffn", "fing", "wr", "rb", "wg", "wu", "wd", "ident",
                             "out", "outn", "x1", "acc", "hTdB", "gates_d"))
    else:
        P = Prog()
        nc = P.nc
        x = P.dram("x", [TPC, D], F32, "ExternalInput")
        yT = P.dram("yT", [D, TPC], BF16, "ExternalInput")
        wout = P.dram("w_out", [D, D], F32, "ExternalInput")
        modv = P.dram("modv", [4, D], F32, "ExternalInput")
        gffn = P.dram("g_ffn", [D], F32, "ExternalInput")
        fing = P.dram("final_g", [D], F32, "ExternalInput")
        wr = P.dram("w_router", [D, NE], F32, "ExternalInput")
        rb = P.dram("router_bias", [NE], F32, "ExternalInput")
        wg = P.dram("w_gate", [NE, D, DE], F32, "ExternalInput")
        wu = P.dram("w_up", [NE, D, DE], F32, "ExternalInput")
        wd = P.dram("w_down", [NE, DE, D], F32, "ExternalInput")
        ident_d = P.dram("ident", [128, 128], F32, "ExternalInput")
        out = P.dram("out", [TPC, D], F32, "ExternalOutput")
        outn = P.dram("outn", [TPC, D], F32, "ExternalOutput")
        x1 = P.dram("x1", [TPC, D], F32, "Internal")
        acc = P.dram("acc", [TPC, D], F32, "Internal")
        hTd = P.dram("hT_scr", [128, KC, TPC], BF16, "Internal")
        gates_d = P.dram("gates_scr", [128, NTT, NE], F32, "Internal")

    P.begin_phase()
    yTs = P.sb([128, KC, TPC], BF16, "yTs")
    P.dma_group([(yTs[:, :16, :], yT.t.rearrange("(k p) t -> p k t", p=128)[:, :16, :]),
                 (yTs[:, 16:, :], yT.t.rearrange("(k p) t -> p k t", p=128)[:, 16:, :])],
                reads=[yT], writes=[yTs], semtrk=yTs)
    gm = P.sb([128, D], F32, "gm")
    P.dma(gm[:], modv.t[0:1, :].partition_broadcast(128), reads=[modv], writes=[gm], semtrk=gm)
    wbs = [P.sb([128, KC, 512], BF16, f"wb{i}") for i in range(2)]
    xios = [P.sb([128, 512], F32, f"xio{i}") for i in range(4)]
    tmps = [P.sb([128, 512], F32, f"tmp{i}") for i in range(2)]
    pss = [P.ps([128, 512], F32, f"ps{i}") for i in range(4)]
    woutv = wout.t.rearrange("(k p) n -> p k n", p=128)
    it = 0
    for ct in range(D // 512):
        cs = slice(ct * 512, (ct + 1) * 512)
        wb = wbs[ct % 2]
        P.dma_group([(wb[:, :16, :], woutv[:, :16, cs]), (wb[:, 16:, :], woutv[:, 16:, cs])],
                    reads=[wout], writes=[wb], semtrk=wb, q="gpsimd")
        for tt in range(NTT):
            ps = pss[it % 4]
            xio = xios[it % 4]
            tmp = tmps[it % 2]
            rs = slice(tt * 128, (tt + 1) * 128)
            P.dma(xio[:], x.t[rs, cs], reads=[x], writes=[xio], semtrk=xio)
            for k in range(KC):
                P.mm(ps[:], yTs[:, k, rs], wb[:, k, :], k == 0, k == KC - 1, [yTs, wb], [ps])
            P.tt(tmp[:], ps[:], gm[:, cs], ALU.mult, [ps, gm], [tmp])
            P.tt(xio[:], tmp[:], xio[:], ALU.add, [tmp, xio], [xio], eng="gpsimd")
            P.dma(x1.t[rs, cs], xio[:], reads=[xio], writes=[x1.sub(tt)], semtrk=xio)
            it += 1
    P.end_phase()

    P.begin_phase()
    ident = P.sb([128, 128], F32, "ident")
    P.dma(ident[:], ident_d.t, reads=[ident_d], writes=[ident], semtrk=ident)
    gT = P.sb([128, KC], F32, "gT")
    scT = P.sb([128, KC], F32, "scT")
    BvT = P.sb([128, KC], F32, "BvT")
    AT = P.sb([128, KC], F32, "AT")
    P.dma(gT[:], gffn.t.rearrange("(k p) -> p k", p=128), reads=[gffn], writes=[gT], semtrk=gT,
          allow_slow_non_contiguous=True)
    P.dma(scT[:], modv.t[2].rearrange("(k p) -> p k", p=128), reads=[modv], writes=[scT], semtrk=scT,
          allow_slow_non_contiguous=True)
    P.dma(BvT[:], modv.t[1].rearrange("(k p) -> p k", p=128), reads=[modv], writes=[BvT], semtrk=BvT,
          allow_slow_non_contiguous=True)
    P.stt(AT[:], scT[:], 1.0, gT[:], ALU.add, ALU.mult, [scT, gT], [AT])
    wrs = P.sb([128, KC, NE], F32, "wrs")
    P.dma(wrs[:], wr.t.rearrange("(k p) e -> p k e", p=128), reads=[wr], writes=[wrs], semtrk=wrs)
    rbb = P.sb([128, NE], F32, "rbb")
    P.dma(rbb[:], rb.t.rearrange("(o e) -> o e", o=1).partition_broadcast(128), reads=[rb], writes=[rbb],
          semtrk=rbb)
    xrs = [P.sb([128, D], F32, f"xr{i}") for i in range(2)]
    junk = P.sb([128, D], BF16, "junk")
    xs = P.sb([128, D], F32, "xs")
    h32 = P.sb([128, KC, 128], F32, "h32")
    hTt = [P.sb([128, KC, 128], BF16, f"hTt{i}") for i in range(2)]
    gates = P.sb([128, NTT, NE], F32, "gates")
    sm = {n: P.sb([128, 1], F32, n) for n in ("ss", "ms", "rstd", "gmax", "den", "rden")}
    r16 = {n: P.sb([128, NE], F32, n) for n in ("sc", "sel", "eq", "sel2", "in2", "selm", "gsel")}
    r4 = {n: P.sb([128, 4], F32, n) for n in ("m1", "m2", "gs", "gmask")}
    pst = [P.ps([128, 512], F32, f"pst{i}") for i in range(4)]
    psr = P.ps([128, NE], F32, "psr")

    def v3(t):
        return t[:].rearrange("p (g e) -> p g e", g=4)

    def b3(t):
        return t[:].unsqueeze(2).to_broadcast([128, 4, 4])

    for tt in range(NTT):
        xr = xrs[tt % 2]
        rs = slice(tt * 128, (tt + 1) * 128)
        P.dma(xr[:], x1.t[rs, :], reads=[x1.sub(tt)], writes=[xr], semtrk=xr)
        P.act(junk[:], xr[:], AF.Square, [xr], [junk, sm["ss"]], accum_out=sm["ss"][:])
        P.ts(sm["ms"][:], sm["ss"][:], 1.0 / D, EPS, ALU.mult, ALU.add, [sm["ss"]], [sm["ms"]])
        P.act(sm["ms"][:], sm["ms"][:], AF.Sqrt, [sm["ms"]], [sm["ms"]])
        P.op("vector", lambda e: e.reciprocal(sm["rstd"][:], sm["ms"][:]), [sm["ms"]], [sm["rstd"]])
        P.act(xs[:], xr[:], AF.Identity, [xr, sm["rstd"]], [xs], scale=sm["rstd"][:])
        for kq in range(KC // 4):
            ps = pst[kq % 4]
            for j in range(4):
                k = kq * 4 + j
                P.op("tensor", lambda e, ps=ps, j=j, k=k: e.transpose(ps[:, j * 128:(j + 1) * 128],
                                                                     xs[:, k * 128:(k + 1) * 128], ident[:]),
                     [xs, ident], [ps])
            for j in range(4):
                k = kq * 4 + j
                P.ts(h32[:, k, :], ps[:, j * 128:(j + 1) * 128], AT[:, k:k + 1], BvT[:, k:k + 1], ALU.mult,
                     ALU.add, [ps, AT, BvT], [h32], eng="vector")
        hb = hTt[tt % 2]
        P.act(hb[:], h32[:], AF.Copy, [h32], [hb])
        P.dma(hTd.t[:, :, rs], hb[:], reads=[hb], writes=[hTd.sub(tt)], semtrk=hb)
        for k in range(KC):
            P.mm(psr[:], h32[:, k, :], wrs[:, k, :], k == 0, k == KC - 1, [h32, wrs], [psr])
        sc, sel, eq, sel2, in2, selm, gsel = (r16[n] for n in ("sc", "sel", "eq", "sel2", "in2", "selm", "gsel"))
        m1, m2, gs, gmask = (r4[n] for n in ("m1", "m2", "gs", "gmask"))
        P.act(sc[:], psr[:], AF.Sigmoid, [psr], [sc])
        P.tt(sel[:], sc[:], rbb[:], ALU.add, [sc, rbb], [sel])
        P.op("vector", lambda e, sel=sel, m1=m1: e.tensor_reduce(m1[:], v3(sel), AX.X, ALU.max), [sel], [m1])
        P.tt(v3(eq), v3(sel), b3(m1), ALU.is_equal, [sel, m1], [eq])
        P.stt(sel2[:], eq[:], -1e30, sel[:], ALU.mult, ALU.add, [eq, sel], [sel2])
        P.op("vector", lambda e, sel2=sel2, m2=m2: e.tensor_reduce(m2[:], v3(sel2), AX.X, ALU.max), [sel2], [m2])
        P.tt(gs[:], m1[:], m2[:], ALU.add, [m1, m2], [gs])
        P.op("vector", lambda e, gs=gs: e.tensor_reduce(sm["gmax"][:], gs[:], AX.X, ALU.max), [gs], [sm["gmax"]])
        P.tt(gmask[:], gs[:], sm["gmax"][:].to_broadcast([128, 4]), ALU.is_ge, [gs, sm["gmax"]], [gmask])
        P.tt(v3(in2), v3(sel), b3(m2), ALU.is_ge, [sel, m2], [in2])
        P.tt(v3(selm), v3(in2), b3(gmask), ALU.mult, [in2, gmask], [selm])
        P.tt(gsel[:], selm[:], sc[:], ALU.mult, [selm, sc], [gsel])
        P.op("vector", lambda e, gsel=gsel: e.tensor_reduce(sm["den"][:], gsel[:], AX.X, ALU.add), [gsel],
             [sm["den"]])
        P.op("vector", lambda e: e.reciprocal(sm["rden"][:], sm["den"][:]), [sm["den"]], [sm["rden"]])
        P.ts(gates[:, tt, :], gsel[:], sm["rden"][:], None, ALU.mult, None, [gsel, sm["rden"]], [gates])
    P.dma(gates_d.t, gates[:], reads=[gates], writes=[gates_d], semtrk=gates)
    P.end_phase()

    P.begin_phase()
    hT = P.sb([128, KC, TPC], BF16, "hT")
    P.dma_group([(hT[:, :, tt * 128:(tt + 1) * 128], hTd.t[:, :, tt * 128:(tt + 1) * 128]) for tt in range(NTT)],
                reads=[hTd.sub(tt) for tt in range(NTT)], writes=[hT], semtrk=hT)
    gates = P.sb([128, NTT, NE], F32, "gates")
    P.dma(gates[:], gates_d.t, reads=[gates_d], writes=[gates], semtrk=gates)
    wgb = [P.sb([128, KC, 128], BF16, f"wgb{i}") for i in range(2)]
    wub = [P.sb([128, KC, 128], BF16, f"wub{i}") for i in range(2)]
    wdb = [P.sb([128, 8, 512], BF16, f"wdb{i}") for i in range(2)]
    hid = [P.sb([128, 8, TPC], BF16, f"hid{i}") for i in range(2)]
    sgs = [P.sb([128, 512], F32, f"sg{i}") for i in range(2)]
    accio = [P.sb([128, 512], F32, f"accio{i}") for i in range(4)]
    psg = [P.ps([128, 512], F32, f"psg{i}") for i in range(2)]
    psu = [P.ps([128, 512], F32, f"psu{i}") for i in range(2)]
    psd = [P.ps([128, 512], F32, f"psd{i}") for i in range(4)]
    i1 = 0
    i2 = 0
    for e in range(n_exp):
        hd_ = hid[e % 2]
        wgv = wg.t[e].rearrange("(k p) n -> p k n", p=128)
        wuv = wu.t[e].rearrange("(k p) n -> p k n", p=128)
        for hc in range(8):
            wgt = wgb[hc % 2]
            wut = wub[hc % 2]
            hs = slice(hc * 128, (hc + 1) * 128)
            P.dma(wgt[:], wgv[:, :, hs], reads=[wg], writes=[wgt], semtrk=wgt, q="gpsimd")
            P.dma(wut[:], wuv[:, :, hs], reads=[wu], writes=[wut], semtrk=wut, q="gpsimd")
            for th in range(2):
                pg = psg[i1 % 2]
                pu = psu[i1 % 2]
                sg = sgs[i1 % 2]
                tsl = slice(th * 512, (th + 1) * 512)
                for k in range(KC):
                    P.mm(pg[:], wgt[:, k, :], hT[:, k, tsl], k == 0, k == KC - 1, [wgt, hT], [pg])
                for k in range(KC):
                    P.mm(pu[:], wut[:, k, :], hT[:, k, tsl], k == 0, k == KC - 1, [wut, hT], [pu])
                P.act(sg[:], pg[:], AF.Silu, [pg], [sg])
                P.tt(hd_[:, hc, tsl], sg[:], pu[:], ALU.mult, [sg, pu], [hd_])
                i1 += 1
        wdv = wd.t[e].rearrange("(c p) n -> p c n", p=128)
        for ct in range(D // 512):
            cs = slice(ct * 512, (ct + 1) * 512)
            wdt = wdb[ct % 2]
            P.dma(wdt[:], wdv[:, :, cs], reads=[wd], writes=[wdt], semtrk=wdt, q="gpsimd")
            for tt in range(NTT):
                rs = slice(tt * 128, (tt + 1) * 128)
                pd = psd[i2 % 4]
                ai = accio[i2 % 4]
                key = (tt, ct)
                if e > 0:
                    P.dma(ai[:], acc.t[rs, cs], reads=[acc.sub(key)], writes=[ai], semtrk=ai)
                for c in range(8):
                    P.mm(pd[:], hd_[:, c, rs], wdt[:, c, :], c == 0, c == 7, [hd_, wdt], [pd])
                if e > 0:
                    P.stt(ai[:], pd[:], gates[:, tt, e:e + 1], ai[:], ALU.mult, ALU.add, [pd, gates, ai], [ai])
                else:
                    P.ts(ai[:], pd[:], gates[:, tt, e:e + 1], None, ALU.mult, None, [pd, gates], [ai])
                P.dma(acc.t[rs, cs], ai[:], reads=[ai], writes=[acc.sub(key)], semtrk=ai)
                i2 += 1
    P.end_phase()

    P.begin_phase()
    gf = P.sb([128, D], F32, "gf")
    P.dma(gf[:], modv.t[3:4, :].partition_broadcast(128), reads=[modv], writes=[gf], semtrk=gf)
    fg = P.sb([128, D], F32, "fg")
    P.dma(fg[:], fing.t.rearrange("(o n) -> o n", o=1).partition_broadcast(128), reads=[fing], writes=[fg],
          semtrk=fg)
    xa = [P.sb([128, D], F32, f"xa{i}") for i in range(2)]
    ab_ = [P.sb([128, D], F32, f"ab{i}") for i in range(2)]
    junk = P.sb([128, D], BF16, "junk4")
    ss = P.sb([128, 1], F32, "ss4")
    ms = P.sb([128, 1], F32, "ms4")
    rstd = P.sb([128, 1], F32, "rstd4")
    for tt in range(NTT):
        rs = slice(tt * 128, (tt + 1) * 128)
        a = xa[tt % 2]
        b = ab_[tt % 2]
        P.dma(a[:], x1.t[rs, :], reads=[x1.sub(tt)], writes=[a], semtrk=a)
        P.dma(b[:], acc.t[rs, :], reads=[acc.sub((tt, ct)) for ct in range(8)], writes=[b], semtrk=b)
        P.tt(b[:], b[:], gf[:], ALU.mult, [b, gf], [b], eng="gpsimd")
        P.tt(a[:], a[:], b[:], ALU.add, [a, b], [a])
        if out is not None:
            P.dma(out.t[rs, :], a[:], reads=[a], writes=[out], semtrk=a)
        if outn is None:
            continue
        P.act(junk[:], a[:], AF.Square, [a], [junk, ss], accum_out=ss[:])
        P.ts(ms[:], ss[:], 1.0 / D, EPS, ALU.mult, ALU.add, [ss], [ms])
        P.act(ms[:], ms[:], AF.Sqrt, [ms], [ms])
        P.op("vector", lambda e: e.reciprocal(rstd[:], ms[:]), [ms], [rstd])
        P.act(b[:], a[:], AF.Identity, [a, rstd], [b], scale=rstd[:])
        P.tt(b[:], b[:], fg[:], ALU.mult, [b, fg], [b], eng="gpsimd")
        P.dma(outn.t[rs, :], b[:], reads=[b], writes=[outn], semtrk=b)
    P.end_phase()
    if ctx is not None:
        return None
    return P.finish()


def run_B(l, last, x, yT_full, mod, inp, n_exp=NE):
    nc = build_B(n_exp)
    ident = np.eye(128, dtype=np.float32)
    in_maps = []
    for i in range(8):
        b = i // 4
        m = mod[l, b].reshape(6, D)
        modv = np.ascontiguousarray(np.stack([m[2], m[3], m[4], m[5]]))
        in_maps.append({
            "x": np.ascontiguousarray(x[i * TPC:(i + 1) * TPC]),
            "yT": np.ascontiguousarray(yT_full[i]),
            "w_out": inp["w_out"][l], "modv": modv, "g_ffn": inp["norm_ffn_g"][l],
            "final_g": inp["final_g"], "w_router": inp["w_router"], "router_bias": inp["router_bias"],
            "w_gate": inp["w_gate_e"][l], "w_up": inp["w_up_e"][l], "w_down": inp["w_down_e"][l],
            "ident": ident,
        })
    res = run_bass_kernel_spmd(nc, in_maps, core_ids=list(range(8)))
    return np.concatenate([r["outn" if last else "out"] for r in res.results], axis=0)


NFM = 18
WC = NFM * 128 + 3 + 384 + 384 + 6
TMW = 774
CT_TOK = 1152


def emit_hT(P, x_d, ntiles, hT_d, AT, BvT, ident, bufs, rowsrc=None):
    xrs, junks, xss, hbs, pst, sss, mss, rstds = bufs
    for tt in range(ntiles):
        xr = xrs[tt % 2]
        junk, xs, ss, ms, rstd = junks[tt % 2], xss[tt % 2], sss[tt % 2], mss[tt % 2], rstds[tt % 2]
        rs = slice(tt * 128, (tt + 1) * 128)
        if rowsrc is None:
            P.dma(xr[:], x_d.t[rs, :], reads=[x_d], writes=[xr], semtrk=xr)
        else:
            P.dma_group([(xr[p0:p1, :], ap) for (p0, p1, ap) in rowsrc(tt)], reads=[x_d], writes=[xr], semtrk=xr)
        P.act(junk[:], xr[:], AF.Square, [xr], [junk, ss], accum_out=ss[:])
        P.ts(ms[:], ss[:], 1.0 / D, EPS, ALU.mult, ALU.add, [ss], [ms])
        P.act(ms[:], ms[:], AF.Sqrt, [ms], [ms])
        P.op("vector", lambda e, rstd=rstd, ms=ms: e.reciprocal(rstd[:], ms[:]), [ms], [rstd])
        P.act(xs[:], xr[:], AF.Identity, [xr, rstd], [xs], scale=rstd[:])
        hb = hbs[tt % 2]
        for kq in range(KC // 4):
            ps = pst[kq % 4]
            for j in range(4):
                k = kq * 4 + j
                P.op("tensor", lambda e, ps=ps, j=j, k=k, xs=xs: e.transpose(ps[:, j * 128:(j + 1) * 128],
                                                                            xs[:, k * 128:(k + 1) * 128], ident[:]),
                     [xs, ident], [ps])
            for j in range(4):
                k = kq * 4 + j
                if j % 2 == 0:
                    P.ts(hb[:, k, :], ps[:, j * 128:(j + 1) * 128], AT[:, k:k + 1], BvT[:, k:k + 1], ALU.mult,
                         ALU.add, [ps, AT, BvT], [hb])
                else:
                    P.act(hb[:, k, :], ps[:, j * 128:(j + 1) * 128], AF.Identity, [ps, AT, BvT], [hb],
                          scale=AT[:, k:k + 1], bias=BvT[:, k:k + 1])
        P.dma(hT_d.t[:, :, rs], hb[:], reads=[hb], writes=[hT_d.sub(tt // 4)], semtrk=hb)


def build_A(stop=9, ctx=None):
    IN, OUT, SCR = "ExternalInput", "ExternalOutput", "Internal"
    if ctx is not None:
        P = ctx["P"]
        nc = P.nc
        (x_b, x_c, wc, wa, modm, gmix, cwT, cvec, halo, hv, fb3, dnp, dcw, ident_d, ut_d, cm_d, ybT, ycT, yaT,
         hTd, hTc, projT, ffd, Fd, projM, aT) = (
            ctx[k] for k in ("x_b", "x_c", "wc", "wa", "modm", "gmix", "cwT", "cvec", "halo", "hv", "fb3", "dnp",
                             "dcw", "ident", "ut", "cmask", "ybT", "ycT", "yaT", "hTd", "hTc", "projT", "ffd",
                             "Fd", "projM", "aT"))
    else:
        P = Prog()
        nc = P.nc
        x_b = P.dram("x_b", [SEQ, D], F32, IN)
        x_c = P.dram("x_c", [CT_TOK, D], F32, IN)
        wc = P.dram("wc", [D, WC], F32, IN)
        wa = P.dram("wa", [D, 2048], F32, IN)
        modm = P.dram("modm", [2, D], F32, IN)
        gmix = P.dram("g_mix", [D], F32, IN)
        cwT = P.dram("cwT", [CONV_CH, 31], F32, IN)
        cvec = P.dram("cvec", [3, CONV_CH], F32, IN)
        halo = P.dram("halo", [128, 1], F32, IN)
        hv = P.dram("hv", [4, 128], F32, IN)
        fb3 = P.dram("fb3", [3, 1], F32, IN)
        dnp = P.dram("dnp", [2, 4], F32, IN)
        dcw = P.dram("dcw", [9 * 128, 4], F32, IN)
        ident_d = P.dram("ident", [128, 128], F32, IN)
        ut_d = P.dram("ut", [128, 128], F32, IN)
        cm_d = P.dram("cmask", [128, 4, 512], BF16, IN)
        ybT = P.dram("ybT", [384, SEQ], BF16, OUT)
        ycT = P.dram("ycT", [384, SEQ], BF16, OUT)
        yaT = P.dram("yaT", [CONV_CH, 1024], BF16, OUT)
        hTd = P.dram("hTd", [128, KC, SEQ], BF16, SCR)
        hTc = P.dram("hTc", [128, KC, CT_TOK], BF16, SCR)
        projT = P.dram("projT", [NFM, 128, SEQ], F32, SCR)
        ffd = P.dram("ffd", [3, SEQ], F32, SCR)
        Fd = P.dram("Fd", [3, SEQ], F32, SCR)
        projM = P.dram("projM", [SEQ, TMW], F32, SCR)
        aT = P.dram("aT", [16, 128, CT_TOK], F32, SCR)

    P.begin_phase()
    ident = P.sb([128, 128], F32, "ident")
    P.dma(ident[:], ident_d.t, reads=[ident_d], writes=[ident], semtrk=ident)
    gT = P.sb([128, KC], F32, "gT")
    scT = P.sb([128, KC], F32, "scT")
    BvT = P.sb([128, KC], F32, "BvT")
    AT = P.sb([128, KC], F32, "AT")
    P.dma(gT[:], gmix.t.rearrange("(k p) -> p k", p=128), reads=[gmix], writes=[gT], semtrk=gT,
          allow_slow_non_contiguous=True)
    P.dma(scT[:], modm.t[1].rearrange("(k p) -> p k", p=128), reads=[modm], writes=[scT], semtrk=scT,
          allow_slow_non_contiguous=True)
    P.dma(BvT[:], modm.t[0].rearrange("(k p) -> p k", p=128), reads=[modm], writes=[BvT], semtrk=BvT,
          allow_slow_non_contiguous=True)
    P.stt(AT[:], scT[:], 1.0, gT[:], ALU.add, ALU.mult, [scT, gT], [AT])
    bufs = ([P.sb([128, D], F32, f"xr{i}") for i in range(2)], [P.sb([128, D], BF16, f"junk{i}") for i in range(2)],
            [P.sb([128, D], F32, f"xs{i}") for i in range(2)], [P.sb([128, KC, 128], BF16, f"hb{i}") for i in range(2)],
            [P.ps([128, 512], F32, f"pst{i}") for i in range(4)],
            [P.sb([128, 1], F32, f"ss{i}") for i in range(2)], [P.sb([128, 1], F32, f"ms{i}") for i in range(2)],
            [P.sb([128, 1], F32, f"rstd{i}") for i in range(2)])
    emit_hT(P, x_b, SEQ // 128, hTd, AT, BvT, ident, bufs, rowsrc=(ctx or {}).get("x_b_rows"))
    emit_hT(P, x_c, CT_TOK // 128, hTc, AT, BvT, ident, bufs)
    P.end_phase()

    if stop < 2:
        return P.finish()
    P.begin_phase()
    wbs = [P.sb([128, KC, 512], BF16, f"wb{i}") for i in range(2)]
    hts = [P.sb([128, KC, 512], BF16, f"ht{i}") for i in range(2)]
    evs = [P.sb([128, 512], F32, f"ev{i}") for i in range(4)]
    pss = [P.ps([128, 512], F32, f"ps{i}") for i in range(4)]
    wcv = wc.t.rearrange("(k p) n -> p k n", p=128)
    wav = wa.t.rearrange("(k p) n -> p k n", p=128)
    groups = []
    for g0 in range(0, NFM, 4):
        groups.append(("FM", g0 * 128, min(4, NFM - g0) * 128, g0))
    groups.append(("FF", NFM * 128, 3, 0))
    groups.append(("TM", NFM * 128 + 3, 384, 0))
    groups.append(("TM", NFM * 128 + 3 + 384, 390, 384))
    cnt = {"w": 0, "h": 0, "e": 0}

    def evac(ps_ap, n_part, ncols, dst_ap, dst_trk):
        ev = evs[cnt["e"] % 4]
        if cnt["e"] % 2 == 0:
            P.act(ev[:n_part, :ncols], ps_ap, AF.Copy, [pss[cnt["p"] % 4]], [ev])
        else:
            P.copy(ev[:n_part, :ncols], ps_ap, [pss[cnt["p"] % 4]], [ev], eng="vector")
        P.dma(dst_ap, ev[:n_part, :ncols], reads=[ev], writes=[dst_trk], semtrk=ev)
        cnt["e"] += 1

    cnt["p"] = 0
    for kind, c0, ncols, aux in groups:
        wb = wbs[cnt["w"] % 2]
        cnt["w"] += 1
        P.dma_group([(wb[:, :16, :ncols], wcv[:, :16, c0:c0 + ncols]), (wb[:, 16:, :ncols], wcv[:, 16:, c0:c0 + ncols])],
                    reads=[wc], writes=[wb], semtrk=wb, q="gpsimd")
        for t in range(SEQ // 512):
            ht = hts[cnt["h"] % 2]
            cnt["h"] += 1
            tsl = slice(t * 512, (t + 1) * 512)
            P.dma(ht[:], hTd.t[:, :, tsl], reads=[hTd.sub(t)], writes=[ht], semtrk=ht)
            if kind == "FM":
                for ui in range(ncols // 128):
                    ps = pss[cnt["p"] % 4]
                    for k in range(KC):
                        P.mm(ps[:], wb[:, k, ui * 128:(ui + 1) * 128], ht[:, k, :], k == 0, k == KC - 1, [wb, ht], [ps])
                    evac(ps[:], 128, 512, projT.t[aux + ui, :, tsl], projT.sub((aux + ui, t)))
                    cnt["p"] += 1
            elif kind == "FF":
                ps = pss[cnt["p"] % 4]
                for k in range(KC):
                    P.mm(ps[:3, :], wb[:, k, 0:3], ht[:, k, :], k == 0, k == KC - 1, [wb, ht], [ps])
                evac(ps[:3, :], 3, 512, ffd.t[:, tsl], ffd)
                cnt["p"] += 1
            else:
                for s4 in range(4):
                    ps = pss[cnt["p"] % 4]
                    for k in range(KC):
                        P.mm(ps[:, :ncols], ht[:, k, s4 * 128:(s4 + 1) * 128], wb[:, k, :ncols], k == 0, k == KC - 1,
                             [wb, ht], [ps])
                    r0 = t * 512 + s4 * 128
                    evac(ps[:, :ncols], 128, ncols, projM.t[r0:r0 + 128, aux:aux + ncols], projM.sub((aux, t)))
                    cnt["p"] += 1
    for g in range(4):
        wb = wbs[cnt["w"] % 2]
        cnt["w"] += 1
        c0 = g * 512
        P.dma_group([(wb[:, :16, :], wav[:, :16, c0:c0 + 512]), (wb[:, 16:, :], wav[:, 16:, c0:c0 + 512])],
                    reads=[wa], writes=[wb], semtrk=wb, q="gpsimd")
        for t in range(3):
            ht = hts[cnt["h"] % 2]
            cnt["h"] += 1
            tsl = slice(t * 384, (t + 1) * 384)
            P.dma(ht[:, :, :384], hTc.t[:, :, tsl], reads=[hTc.sub(i) for i in range(3)], writes=[ht], semtrk=ht)
            for ui in range(4):
                ps = pss[cnt["p"] % 4]
                for k in range(KC):
                    P.mm(ps[:, :384], wb[:, k, ui * 128:(ui + 1) * 128], ht[:, k, :384], k == 0, k == KC - 1, [wb, ht], [ps])
                evac(ps[:, :384], 128, 384, aT.t[g * 4 + ui, :, tsl], aT.sub(g * 4 + ui))
                cnt["p"] += 1
    P.end_phase()

    if stop < 3:
        return P.finish()
    P.begin_phase()
    ones32 = P.sb([128, 128], F32, "ones32")
    P.memset(ones32[:], 1.0, [ones32])
    cw = P.sb([128, 8, 31], F32, "cw")
    P.dma(cw[:], cwT.t.rearrange("(u p) j -> p u j", p=128), reads=[cwT], writes=[cw], semtrk=cw)
    cv3 = P.sb([128, 3, 8], F32, "cv3")
    P.dma_group([(cv3[:, i, :], cvec.t[i].rearrange("(u p) -> p u", p=128)) for i in range(3)],
                reads=[cvec], writes=[cv3], semtrk=cv3, allow_slow_non_contiguous=True)
    hal = P.sb([128, 1], F32, "hal")
    P.dma(hal[:], halo.t, reads=[halo], writes=[hal], semtrk=hal)
    cvs = [P.sb([128, 1024], F32, f"cv{u}") for u in range(8)]
    avs = [P.sb([128, CT_TOK], F32, f"av{i}") for i in range(2)]
    ags = [P.sb([128, CT_TOK], F32, f"ag{i}") for i in range(2)]
    acc2 = P.sb([128, 1024], F32, "acc2")
    sq = P.sb([128, 512], F32, "sq")
    stat = {n: P.sb([128, 1024], F32, n) for n in ("mean", "rstd", "t1")}
    yo = [P.sb([128, 1024], BF16, f"yo{i}") for i in range(2)]
    psS = [P.ps([128, 512], F32, f"psS{i}") for i in range(2)]
    psQ = [P.ps([128, 512], F32, f"psQ{i}") for i in range(2)]
    for u in range(8):
        av = avs[u % 2]
        ag = ags[u % 2]
        P.dma(av[:], aT.t[u], reads=[aT.sub(u)], writes=[av], semtrk=av)
        P.dma(ag[:], aT.t[8 + u], reads=[aT.sub(8 + u)], writes=[ag], semtrk=ag)
        P.act(ag[:], ag[:], AF.Sigmoid, [ag], [ag])
        P.tt(av[:], av[:], ag[:], ALU.mult, [av, ag], [av])
        P.ts(av[:, :128], av[:, :128], hal[:], None, ALU.mult, None, [av, hal], [av])
        cvu = cvs[u]
        P.ts(cvu[:], av[:, 98:98 + 1024], cw[:, u, 0:1], cv3[:, 0, u:u + 1], ALU.mult, ALU.add, [av, cw, cv3], [cvu])
        for j in range(1, 31):
            P.stt(cvu[:], av[:, 98 + j:98 + j + 1024], cw[:, u, j:j + 1], cvu[:], ALU.mult, ALU.add,
                  [av, cw, cvu], [cvu])
    for th in range(2):
        tsl = slice(th * 512, (th + 1) * 512)
        for u in range(8):
            P.mm(psS[th][:], ones32[:], cvs[u][:, tsl], u == 0, u == 7, [ones32, cvs[u]], [psS[th]])
        for u in range(8):
            P.act(sq[:], cvs[u][:, tsl], AF.Square, [cvs[u]], [sq])
            P.mm(psQ[th][:], ones32[:], sq[:], u == 0, u == 7, [ones32, sq], [psQ[th]])
        mean, rstd, t1 = stat["mean"], stat["rstd"], stat["t1"]
        P.ts(mean[:, tsl], psS[th][:], 1.0 / CONV_CH, None, ALU.mult, None, [psS[th]], [mean])
        P.tt(t1[:, tsl], mean[:, tsl], mean[:, tsl], ALU.mult, [mean], [t1])
        P.stt(t1[:, tsl], psQ[th][:], 1.0 / CONV_CH, t1[:, tsl], ALU.mult, ALU.subtract, [psQ[th], t1], [t1])
        P.ts(t1[:, tsl], t1[:, tsl], EPS, None, ALU.add, None, [t1], [t1])
        P.act(t1[:, tsl], t1[:, tsl], AF.Sqrt, [t1], [t1])
        P.op("vector", lambda e, tsl=tsl: e.reciprocal(rstd[:, tsl], t1[:, tsl]), [t1], [rstd])
    for u in range(8):
        t1 = stat["t1"]
        y = yo[u % 2]
        P.tt(t1[:], cvs[u][:], stat["mean"][:], ALU.subtract, [cvs[u], stat["mean"]], [t1])
        P.tt(t1[:], t1[:], stat["rstd"][:], ALU.mult, [t1, stat["rstd"]], [t1], eng="gpsimd")
        P.act(y[:], t1[:], AF.Silu, [t1, cv3], [y], scale=cv3[:, 1, u:u + 1], bias=cv3[:, 2, u:u + 1])
        P.dma(yaT.t[u * 128:(u + 1) * 128, :], y[:], reads=[y], writes=[yaT], semtrk=y)
    P.end_phase()

    import os
    if stop >= 4 and not os.environ.get("A_SKIPFOX"):
        build_fox(P, projT, projM, ffd, Fd, hv, fb3, cm_d, ybT)
    if stop >= 5:
        build_dn(P, projT, projM, hv, dnp, dcw, ident_d, ut_d, ycT)
    if ctx is not None:
        return None
    return P.finish()


def build_fox(P, projT, projM, ffd, Fd, hv, fb3, cm_d, ybT):
    P.begin_phase()
    ones32 = P.sb([128, 128], F32, "ones32")
    P.memset(ones32[:], 1.0, [ones32])
    onesb = P.sb([128, 128], BF16, "onesb")
    P.memset(onesb[:], 1.0, [onesb])
    cmask = P.sb([128, 4, 512], BF16, "cmask")
    P.dma(cmask[:], cm_d.t, reads=[cm_d], writes=[cmask], semtrk=cmask)
    gains = P.sb([128, 4], F32, "gains")
    P.dma(gains[:], hv.t.rearrange("r p -> p r"), reads=[hv], writes=[gains], semtrk=gains,
          allow_slow_non_contiguous=True)
    fa = P.sb([3, SEQ], F32, "fa")
    fbuf = P.sb([3, SEQ], F32, "fbuf")
    nb = P.sb([3, 1], F32, "nb")
    P.dma(fa[:], ffd.t, reads=[ffd], writes=[fa], semtrk=fa)
    P.dma(nb[:], fb3.t, reads=[fb3], writes=[nb], semtrk=nb)
    P.ts(nb[:], nb[:], -1.0, None, ALU.mult, None, [nb], [nb])
    P.act(fa[:], fa[:], AF.Exp, [fa, nb], [fa], bias=nb[:], scale=-1.0)
    P.act(fa[:], fa[:], AF.Ln, [fa], [fa], bias=1.0)
    a, b = fa, fbuf
    s = 1
    while s < SEQ:
        P.tt(b[:, s:], a[:, s:], a[:, :SEQ - s], ALU.add, [a], [b])
        P.copy(b[:, :s], a[:, :s], [a], [b], eng="gpsimd")
        a, b = b, a
        s *= 2
    P.ts(b[:], a[:], -1.0, None, ALU.mult, None, [a], [b])
    P.dma(Fd.t, b[:], reads=[b], writes=[Fd], semtrk=b)
    FT = P.sb([128, 3, 32], F32, "FT")
    Fref = P.sb([128, 3, 32], F32, "Fref")
    P.dma_group([(FT[:, h, :], Fd.t[h].rearrange("(kt p) -> p kt", p=128)) for h in range(3)],
                reads=[Fd], writes=[FT], semtrk=FT, allow_slow_non_contiguous=True)
    P.dma_group([(Fref[:, h, :], Fd.t[h:h + 1, :].rearrange("o (g s) -> o g s", s=128)[:, :, 127].partition_broadcast(128))
                 for h in range(3)], reads=[Fd], writes=[Fref], semtrk=Fref, allow_slow_non_contiguous=True)
    biasT = P.sb([128, 3, 32, 32], F32, "biasT")
    for h in range(3):
        for g in range(32):
            P.ts(biasT[:, h, g, :], FT[:, h, :], -1.0, Fref[:, h, g:g + 1], ALU.mult, ALU.add, [FT, Fref], [biasT])

    qn = P.sb([128, SEQ], BF16, "qn")
    kn = P.sb([128, SEQ], BF16, "kn")
    v32 = P.sb([128, 32, 128], F32, "v32")
    vb = P.sb([128, 32, 128], BF16, "vb")
    xt = [P.sb([128, 512], F32, f"xt{i}") for i in range(2)]
    sqt = [P.sb([128, 512], F32, f"sqt{i}") for i in range(2)]
    rt = [P.sb([128, 512], F32, f"rt{i}") for i in range(2)]
    pT = [P.sb([128, 512], BF16, f"pT{i}") for i in range(3)]
    ot = [P.sb([128, 512], F32, f"ot{i}") for i in range(2)]
    fgt = [P.sb([128, 512], F32, f"fgt{i}") for i in range(2)]
    yt = [P.sb([128, 512], BF16, f"yt{i}") for i in range(2)]
    ps_s = [P.ps([128, 512], F32, f"ps_s{i}") for i in range(3)]
    po = [P.ps([128, 512], F32, f"po{i}") for i in range(2)]
    pden = [P.ps([128, 512], F32, f"pden{i}") for i in range(2)]
    ps_n = P.ps([128, 512], F32, "ps_n")
    ci = 0
    gi = 0
    scale = float(HD) ** -0.5
    for h in range(3):
        for comp, dst, gcol in ((h, qn, 0), (3 + h, kn, 1)):
            for t in range(8):
                tsl = slice(t * 512, (t + 1) * 512)
                x_ = xt[ci % 2]
                sq_ = sqt[ci % 2]
                r_ = rt[ci % 2]
                ci += 1
                P.dma(x_[:], projT.t[comp, :, tsl], reads=[projT.sub((comp, t))], writes=[x_], semtrk=x_)
                P.act(sq_[:], x_[:], AF.Square, [x_], [sq_])
                P.mm(ps_n[:], ones32[:], sq_[:], True, True, [ones32, sq_], [ps_n])
                P.ts(r_[:], ps_n[:], 1.0 / HD, EPS, ALU.mult, ALU.add, [ps_n], [r_])
                P.act(r_[:], r_[:], AF.Sqrt, [r_], [r_])
                P.op("vector", lambda e, r_=r_: e.reciprocal(r_[:], r_[:]), [r_], [r_])
                P.tt(x_[:], x_[:], r_[:], ALU.mult, [x_, r_], [x_], eng="gpsimd")
                P.ts(dst[:, tsl], x_[:], gains[:, gcol:gcol + 1], None, ALU.mult, None, [x_, gains], [dst])
        P.dma(v32[:], projM.t[:, h * 128:(h + 1) * 128].rearrange("(kt p) c -> p kt c", p=128),
              reads=[projM.sub((0, t)) for t in range(8)], writes=[v32], semtrk=v32)
        P.copy(vb[:], v32[:], [v32], [vb], eng="gpsimd")
        for g in range(8):
            gsl = slice(g * 512, (g + 1) * 512)
            po_, pd_ = po[gi % 2], pden[gi % 2]
            nkt = 4 * g + 4
            for kt in range(nkt):
                pss_ = ps_s[kt % 3]
                p_ = pT[kt % 3]
                P.mm(pss_[:], kn[:, kt * 128:(kt + 1) * 128], qn[:, gsl], True, True, [kn, qn], [pss_])
                jd = kt - 4 * g
                for qq in range(4):
                    qs = slice(qq * 128, (qq + 1) * 128)
                    if qq < jd:
                        P.memset(p_[:, qs], 0.0, [p_], eng="gpsimd")
                        continue
                    P.act(p_[:, qs], pss_[:, qs], AF.Exp, [pss_, biasT], [p_], bias=biasT[:, h, 4 * g + qq, kt:kt + 1],
                          scale=scale)
                    if qq == jd:
                        P.tt(p_[:, qs], p_[:, qs], cmask[:, 0, 0:128], ALU.mult, [p_, cmask], [p_], eng="gpsimd")
                P.mm(po_[:], vb[:, kt, :], p_[:], kt == 0, kt == nkt - 1, [vb, p_], [po_])
                P.mm(pd_[:], onesb[:], p_[:], kt == 0, kt == nkt - 1, [onesb, p_], [pd_])
            o_ = ot[gi % 2]
            r_ = rt[gi % 2]
            sq_ = sqt[gi % 2]
            fg_ = fgt[gi % 2]
            y_ = yt[gi % 2]
            P.dma(fg_[:], projT.t[6 + h, :, gsl], reads=[projT.sub((6 + h, g))], writes=[fg_], semtrk=fg_)
            P.op("vector", lambda e, r_=r_, pd_=pd_: e.reciprocal(r_[:], pd_[:]), [pd_], [r_])
            P.tt(o_[:], po_[:], r_[:], ALU.mult, [po_, r_], [o_])
            P.act(sq_[:], o_[:], AF.Square, [o_], [sq_])
            P.mm(ps_n[:], ones32[:], sq_[:], True, True, [ones32, sq_], [ps_n])
            P.ts(r_[:], ps_n[:], 1.0 / HD, EPS, ALU.mult, ALU.add, [ps_n], [r_])
            P.act(r_[:], r_[:], AF.Sqrt, [r_], [r_])
            P.op("vector", lambda e, r_=r_: e.reciprocal(r_[:], r_[:]), [r_], [r_])
            P.act(fg_[:], fg_[:], AF.Sigmoid, [fg_], [fg_])
            P.tt(o_[:], o_[:], r_[:], ALU.mult, [o_, r_], [o_])
            P.tt(o_[:], o_[:], fg_[:], ALU.mult, [o_, fg_], [o_], eng="gpsimd")
            P.ts(y_[:], o_[:], gains[:, 2:3], None, ALU.mult, None, [o_, gains], [y_])
            P.dma(ybT.t[h * 128:(h + 1) * 128, gsl], y_[:], reads=[y_], writes=[ybT], semtrk=y_)
            gi += 1
    P.end_phase()


def build_dn(P, projT, projM, hv, dnp, dcw, ident_d, ut_d, ycT):
    import os
    DS = float(os.environ.get("DN_STOP", "99"))
    P.begin_phase()
    NCH = SEQ // 128
    ident = P.sb([128, 128], F32, "ident")
    ut = P.sb([128, 128], F32, "ut")
    P.dma(ident[:], ident_d.t, reads=[ident_d], writes=[ident], semtrk=ident)
    P.dma(ut[:], ut_d.t, reads=[ut_d], writes=[ut], semtrk=ut)
    ones32 = P.sb([128, 128], F32, "ones32")
    P.memset(ones32[:], 1.0, [ones32])
    gon = P.sb([128, 128], F32, "gon")
    P.dma(gon[:], hv.t[3:4, :].partition_broadcast(128), reads=[hv], writes=[gon], semtrk=gon)
    dcwt = P.sb([128, 9, 4], F32, "dcwt")
    P.dma(dcwt[:], dcw.t.rearrange("(u p) j -> p u j", p=128), reads=[dcw], writes=[dcwt], semtrk=dcwt)
    pbk = [P.ps([128, 512], F32, f"pb{i}") for i in range(4)]
    pmisc = P.ps([128, 512], F32, "pmisc")
    pscan = [P.ps([128, 512], F32, f"pscan{i}") for i in range(2)]

    def slot(pb, s):
        return pb[:, s * 128:(s + 1) * 128]

    Lm = P.sb([128, 128], F32, "Lm")
    SLm = P.sb([128, 128], F32, "SLm")
    P.op("tensor", lambda e: e.transpose(slot(pmisc, 0), ut[:], ident[:]), [ut, ident], [pmisc])
    P.copy(Lm[:], slot(pmisc, 0), [pmisc], [Lm])
    P.tt(SLm[:], Lm[:], ident[:], ALU.subtract, [Lm, ident], [SLm])
    gb6 = P.sb([128, NCH, 6], F32, "gb6")
    P.dma(gb6[:], projM.t[:, 768:774].rearrange("(c p) n -> p c n", p=128),
          reads=[projM.sub((384, t)) for t in range(8)], writes=[gb6], semtrk=gb6)
    pr = P.sb([128, 2, 4], F32, "pr")
    P.dma_group([(pr[:, i, :], dnp.t[i:i + 1, :].partition_broadcast(128)) for i in range(2)],
                reads=[dnp], writes=[pr], semtrk=pr)
    beta = P.sb([128, NCH, 4], F32, "beta")
    g4 = P.sb([128, NCH, 4], F32, "g4")
    P.memset(g4[:], 0.0, [g4])
    P.memset(beta[:], 0.0, [beta])
    P.act(beta[:, :, 0:3], gb6[:, :, 0:3], AF.Sigmoid, [gb6], [beta])
    zt = P.sb([128, NCH, 3], F32, "zt")
    P.tt(zt[:], gb6[:, :, 3:6], pr[:, 1, 0:3].unsqueeze(1).to_broadcast([128, NCH, 3]), ALU.add, [gb6, pr], [zt])
    P.act(zt[:], zt[:], AF.Exp, [zt], [zt])
    P.act(zt[:], zt[:], AF.Ln, [zt], [zt], bias=1.0)
    ea = P.sb([128, 4], F32, "ea")
    P.act(ea[:], pr[:, 0, :], AF.Exp, [pr], [ea])
    P.ts(ea[:], ea[:], -1.0, None, ALU.mult, None, [ea], [ea])
    P.tt(g4[:, :, 0:3], zt[:], ea[:, 0:3].unsqueeze(1).to_broadcast([128, NCH, 3]), ALU.mult, [zt, ea], [g4])

    if DS <= 1:
        P.end_phase()
        return
    xin = P.sb([128, SEQ], F32, "xin")
    ycv = P.sb([128, SEQ], F32, "ycv")
    qTb = P.sb([128, SEQ], BF16, "qTb")
    kTb = P.sb([128, SEQ], BF16, "kTb")
    kT32 = P.sb([128, SEQ], F32, "kT32")
    vT32 = P.sb([128, SEQ], F32, "vT32")
    sq_ = P.sb([128, 512], F32, "sq")
    r_ = P.sb([128, 512], F32, "r")
    u_all = P.sb([128, NCH, 128], F32, "u_all")
    wT_all = P.sb([128, NCH, 128], BF16, "wT_all")
    qkT_all = P.sb([128, NCH, 128], BF16, "qkT_all")
    kdec_all = P.sb([128, NCH, 128], BF16, "kdec_all")
    egc_all = P.sb([128, NCH], F32, "egc_all")
    egl_all = P.sb([128, NCH], F32, "egl_all")
    NBAT = 4
    W = [{n: P.sb([128, 128], F32, f"{n}{i}") for n in
          ("gbm", "gcb", "E", "A", "qk", "B", "M0", "M1", "A2", "B2", "A3", "B3", "bkg", "bv")} for i in range(NBAT)]
    Wc = [{n: P.sb([128, 1], F32, f"{n}{i}") for n in ("gcc", "edec", "bgc", "tmp")} for i in range(NBAT)]
    S32 = P.sb([128, 128], F32, "S32")
    Sb = P.sb([128, 128], BF16, "Sb")
    vnew = [P.sb([128, 128], BF16, f"vnew{i}") for i in range(2)]
    ot = [P.sb([128, 128], F32, f"ot{i}") for i in range(2)]
    t2 = [P.sb([128, 128], F32, f"t2{i}") for i in range(2)]
    dzt = [P.sb([128, 128], F32, f"dzt{i}") for i in range(2)]
    junk = P.sb([128, 128], F32, "junk")
    sc1 = [{n: P.sb([128, 1], F32, f"{n}{i}") for n in ("ss", "ms", "rstd")} for i in range(2)]
    ybuf = [P.sb([128, 512], BF16, f"ybuf{i}") for i in range(2)]

    for h in range(3):
        for comp in range(3):
            u = comp * 3 + h
            P.dma(xin[:], projT.t[9 + u], reads=[projT.sub((9 + u, t)) for t in range(8)], writes=[xin], semtrk=xin)
            P.ts(ycv[:], xin[:], dcwt[:, u, 3:4], None, ALU.mult, None, [xin, dcwt], [ycv])
            for j in (1, 2, 3):
                P.stt(ycv[:, j:], xin[:, :SEQ - j], dcwt[:, u, 3 - j:4 - j], ycv[:, j:], ALU.mult, ALU.add,
                      [xin, dcwt, ycv], [ycv])
            if comp == 2:
                P.act(vT32[:], ycv[:], AF.Silu, [ycv], [vT32])
                continue
            P.act(ycv[:], ycv[:], AF.Silu, [ycv], [ycv])
            for t in range(8):
                tsl = slice(t * 512, (t + 1) * 512)
                P.act(sq_[:], ycv[:, tsl], AF.Square, [ycv], [sq_])
                P.mm(pmisc[:], ones32[:], sq_[:], True, True, [ones32, sq_], [pmisc])
                P.ts(r_[:], pmisc[:], EPS, None, ALU.add, None, [pmisc], [r_])
                P.act(r_[:], r_[:], AF.Sqrt, [r_], [r_])
                P.op("vector", lambda e: e.reciprocal(r_[:], r_[:]), [r_], [r_])
                if comp == 0:
                    P.stt(qTb[:, tsl], ycv[:, tsl], float(HD) ** -0.5, r_[:], ALU.mult, ALU.mult, [ycv, r_], [qTb])
                else:
                    P.tt(kT32[:, tsl], ycv[:, tsl], r_[:], ALU.mult, [ycv, r_], [kT32])
                    P.copy(kTb[:, tsl], kT32[:, tsl], [kT32], [kTb], eng="gpsimd")
        if DS <= 2:
            P.end_phase()
            return
        for c0 in range(0, NCH, NBAT):
            chains = list(range(c0, c0 + NBAT))

            def each(f):
                for i, c in enumerate(chains):
                    f(i, c, W[i], Wc[i], pbk[i], slice(c * 128, (c + 1) * 128))

            def s1(i, c, w, wc, pb, csl):
                P.ts(w["gbm"][:], ones32[:], g4[:, c, h:h + 1], None, ALU.mult, None, [ones32, g4], [w["gbm"]])
                P.mm(slot(pb, 0), w["gbm"][:], ut[:], True, True, [w["gbm"], ut], [pb])
                P.mm(pb[:, 128:132], ut[:], g4[:, c, :], True, True, [ut, g4], [pb])
                P.mm(slot(pb, 2), kTb[:, csl], kTb[:, csl], True, True, [kTb], [pb])
                P.mm(slot(pb, 3), qTb[:, csl], kTb[:, csl], True, True, [qTb, kTb], [pb])
            each(s1)

            def s2(i, c, w, wc, pb, csl):
                P.copy(w["gcb"][:], slot(pb, 0), [pb], [w["gcb"]], eng="scalar")
                P.copy(wc["gcc"][:], pb[:, 128 + h:129 + h], [pb], [wc["gcc"]])
                P.ts(w["E"][:], w["gcb"][:], -1.0, wc["gcc"][:], ALU.mult, ALU.add, [w["gcb"], wc["gcc"]], [w["E"]])
                P.ts(w["E"][:], w["E"][:], 0.0, None, ALU.min, None, [w["E"]], [w["E"]])
                P.act(w["E"][:], w["E"][:], AF.Exp, [w["E"]], [w["E"]])
                P.tt(wc["tmp"][:], w["gcb"][:, 127:128], wc["gcc"][:], ALU.subtract, [w["gcb"], wc["gcc"]], [wc["tmp"]])
                P.act(wc["edec"][:], wc["tmp"][:], AF.Exp, [wc["tmp"]], [wc["edec"]])
                P.act(egc_all[:, c:c + 1], wc["gcc"][:], AF.Exp, [wc["gcc"]], [egc_all])
                P.act(egl_all[:, c:c + 1], w["gcb"][:, 127:128], AF.Exp, [w["gcb"]], [egl_all])
                P.tt(wc["bgc"][:], beta[:, c, h:h + 1], egc_all[:, c:c + 1], ALU.mult, [beta, egc_all], [wc["bgc"]])
                P.tt(w["A"][:], slot(pb, 2), w["E"][:], ALU.mult, [pb, w["E"]], [w["A"]])
                P.stt(w["A"][:], w["A"][:], beta[:, c, h:h + 1], SLm[:], ALU.mult, ALU.mult, [w["A"], beta, SLm], [w["A"]])
                P.tt(w["qk"][:], slot(pb, 3), w["E"][:], ALU.mult, [pb, w["E"]], [w["qk"]])
                P.tt(w["qk"][:], w["qk"][:], Lm[:], ALU.mult, [w["qk"], Lm], [w["qk"]], eng="gpsimd")
            each(s2)
            if DS <= 3:
                P.end_phase()
                return

            DBG = os.environ.get("DN_DBG", "")

            def s3(i, c, w, wc, pb, csl):
                if "1" in DBG and i > 0:
                    return
                if "A" not in DBG:
                    P.op("tensor", lambda e: e.transpose(slot(pb, 0), w["A"][:], ident[:]), [w["A"], ident], [pb])
                if "Q" not in DBG:
                    P.op("tensor", lambda e: e.transpose(slot(pb, 1), w["qk"][:], ident[:]), [w["qk"], ident], [pb])
            each(s3)
            if DS <= 3.1:
                P.end_phase()
                return

            def s4(i, c, w, wc, pb, csl):
                if "a" not in DBG:
                    P.copy(w["B"][:], slot(pb, 0), [pb], [w["B"]], eng="scalar")
                if "v" not in DBG:
                    P.copy(qkT_all[:, c, :], slot(pb, 1), [pb], [qkT_all])
                if "p" not in DBG:
                    P.tt(w["M0"][:], ident[:], w["B"][:], ALU.subtract, [ident, w["B"]], [w["M0"]], eng="gpsimd")
            each(s4)
            if DS <= 3.3:
                P.end_phase()
                return
            names = [("A", "B", "A2", "B2"), ("A2", "B2", "A3", "B3"), ("A3", "B3", "A2", "B2")]
            for lvl in range(6):
                na, nb_, na2, nb2 = names[0] if lvl == 0 else names[1 + (lvl - 1) % 2]
                mi, mo = ("M0", "M1") if lvl % 2 == 0 else ("M1", "M0")

                def q1(i, c, w, wc, pb, csl):
                    P.mm(slot(pb, 2), w[nb_][:], w[na][:], True, True, [w[nb_], w[na]], [pb])
                    if lvl < 5:
                        P.mm(slot(pb, 3), w[na][:], w[nb_][:], True, True, [w[nb_], w[na]], [pb])
                each(q1)

                def q2(i, c, w, wc, pb, csl):
                    P.copy(w[na2][:], slot(pb, 2), [pb], [w[na2]], eng="scalar")
                    if lvl < 5:
                        P.copy(w[nb2][:], slot(pb, 3), [pb], [w[nb2]])
                each(q2)

                def q3(i, c, w, wc, pb, csl):
                    P.mm(slot(pb, 0), w[na2][:], w[mi][:], True, True, [w[na2], w[mi]], [pb])
                each(q3)

                def q4(i, c, w, wc, pb, csl):
                    P.tt(w[mo][:], w[mi][:], slot(pb, 0), ALU.add, [w[mi], pb], [w[mo]])
                each(q4)
            if DS <= 3.6:
                P.end_phase()
                return
            mf = "M0"

            def s5(i, c, w, wc, pb, csl):
                P.op("tensor", lambda e: e.transpose(slot(pb, 1), kT32[:, csl], ident[:]), [kT32, ident], [pb])
                P.op("tensor", lambda e: e.transpose(slot(pb, 2), vT32[:, csl], ident[:]), [vT32, ident], [pb])
            each(s5)

            def s6(i, c, w, wc, pb, csl):
                P.ts(w["bkg"][:], slot(pb, 1), wc["bgc"][:], None, ALU.mult, None, [pb, wc["bgc"]], [w["bkg"]])
                P.ts(kdec_all[:, c, :], slot(pb, 1), wc["edec"][:], None, ALU.mult, None, [pb, wc["edec"]], [kdec_all],
                     eng="gpsimd" if False else "vector")
                P.ts(w["bv"][:], slot(pb, 2), beta[:, c, h:h + 1], None, ALU.mult, None, [pb, beta], [w["bv"]])
            each(s6)

            def s7(i, c, w, wc, pb, csl):
                P.mm(slot(pb, 3), w[mf][:], w["bv"][:], True, True, [w[mf], w["bv"]], [pb])
                P.mm(slot(pb, 0), w["bkg"][:], w[mf][:], True, True, [w[mf], w["bkg"]], [pb])
            each(s7)

            def s8(i, c, w, wc, pb, csl):
                P.copy(u_all[:, c, :], slot(pb, 3), [pb], [u_all], eng="scalar")
                P.copy(wT_all[:, c, :], slot(pb, 0), [pb], [wT_all])
            each(s8)
            if DS <= 4:
                P.end_phase()
                return
        if DS <= 5:
            P.end_phase()
            return
        P.memset(S32[:], 0.0, [S32])
        P.memset(Sb[:], 0.0, [Sb], eng="gpsimd")
        for c in range(NCH):
            csl = slice(c * 128, (c + 1) * 128)
            pb = pscan[c % 2]
            vn = vnew[c % 2]
            o_ = ot[c % 2]
            t_ = t2[c % 2]
            dz_ = dzt[c % 2]
            s_ = sc1[c % 2]
            yb = ybuf[(c // 4) % 2]
            P.dma(dz_[:], projM.t[csl, 384 + h * 128:384 + (h + 1) * 128], reads=[projM.sub((384, c // 4))],
                  writes=[dz_], semtrk=dz_)
            P.mm(slot(pb, 0), wT_all[:, c, :], Sb[:], True, True, [wT_all, Sb], [pb])
            P.mm(slot(pb, 1), qTb[:, csl], Sb[:], True, True, [qTb, Sb], [pb])
            P.tt(vn[:], u_all[:, c, :], slot(pb, 0), ALU.subtract, [u_all, pb], [vn])
            P.mm(slot(pb, 2), qkT_all[:, c, :], vn[:], True, True, [qkT_all, vn], [pb])
            P.mm(slot(pb, 3), kdec_all[:, c, :], vn[:], True, True, [kdec_all, vn], [pb])
            P.stt(S32[:], S32[:], egl_all[:, c:c + 1], slot(pb, 3), ALU.mult, ALU.add, [S32, egl_all, pb], [S32])
            P.copy(Sb[:], S32[:], [S32], [Sb], eng="scalar")
            P.act(t_[:], slot(pb, 1), AF.Identity, [pb, egc_all], [t_], scale=egc_all[:, c:c + 1])
            P.tt(o_[:], t_[:], slot(pb, 2), ALU.add, [t_, pb], [o_], eng="vector")
            P.act(junk[:], o_[:], AF.Square, [o_], [junk, s_["ss"]], accum_out=s_["ss"][:])
            P.ts(s_["ms"][:], s_["ss"][:], 1.0 / HD, EPS, ALU.mult, ALU.add, [s_["ss"]], [s_["ms"]], eng="gpsimd")
            P.act(s_["ms"][:], s_["ms"][:], AF.Sqrt, [s_["ms"]], [s_["ms"]])
            P.op("vector", lambda e, s_=s_: e.reciprocal(s_["rstd"][:], s_["ms"][:]), [s_["ms"]], [s_["rstd"]])
            P.act(dz_[:], dz_[:], AF.Silu, [dz_], [dz_])
            P.stt(t_[:], o_[:], s_["rstd"][:], gon[:], ALU.mult, ALU.mult, [o_, s_["rstd"], gon], [t_])
            P.tt(t_[:], t_[:], dz_[:], ALU.mult, [t_, dz_], [t_], eng="gpsimd")
            P.op("tensor", lambda e, t_=t_, c=c: e.transpose(slot(pmisc, c % 4), t_[:], ident[:]), [t_, ident], [pmisc])
            P.copy(yb[:, (c % 4) * 128:(c % 4 + 1) * 128], slot(pmisc, c % 4), [pmisc], [yb], eng="gpsimd" if False else "vector")
            if c % 4 == 3:
                P.dma(ycT.t[h * 128:(h + 1) * 128, (c // 4) * 512:(c // 4 + 1) * 512], yb[:], reads=[yb], writes=[ycT],
                      semtrk=yb)
    P.end_phase()


def host_consts():
    ident = np.eye(128, dtype=np.float32)
    ut = np.triu(np.ones((128, 128), np.float32))
    k = np.arange(128)[:, None, None]
    j = np.arange(4)[None, :, None]
    qq = np.arange(512)[None, None, :]
    cmask = ((j * 128 + k) <= qq).astype(ml_dtypes.bfloat16)
    return ident, ut, cmask


def run_A(l, x3, mod, inp):
    import os
    nc = build_A(int(os.environ.get("A_STOP", "9")))
    ident, ut, cmask = host_consts()
    w_in = inp["w_in"][l]
    in_maps = []
    for i in range(8):
        b, q = i // 4, i % 4
        hs = [3 * q + hh for hh in range(3)]
        cols = []
        for base in (O_FQ, O_FK, O_FG, O_DQ, O_DK, O_DV):
            for hh in hs:
                cols.append(np.arange(base + hh * 128, base + (hh + 1) * 128))
        cols.append(np.array([O_FF + hh for hh in hs]))
        for base in (O_FV, O_DZ):
            for hh in hs:
                cols.append(np.arange(base + hh * 128, base + (hh + 1) * 128))
        cols.append(np.array([O_DB + hh for hh in hs]))
        cols.append(np.array([O_DA + hh for hh in hs]))
        cols = np.concatenate(cols)
        assert cols.size == WC
        xc = np.zeros((CT_TOK, D), np.float32)
        if q == 0:
            xc[128:] = x3[b, :1024]
        else:
            xc[:] = x3[b, q * 1024 - 128:(q + 1) * 1024]
        m = mod[l, b].reshape(6, D)
        dcw_rows = []
        for comp in range(3):
            for hh in hs:
                dcw_rows.append(inp["dn_conv_w"][l][:, comp * DN_W + hh * 128: comp * DN_W + (hh + 1) * 128].T)
        dnp = np.zeros((2, 4), np.float32)
        dnp[0, :3] = inp["dn_a_log"][l][hs]
        dnp[1, :3] = inp["dn_dt_bias"][l][hs]
        in_maps.append({
            "x_b": np.ascontiguousarray(x3[b]), "x_c": xc,
            "wc": np.ascontiguousarray(w_in[:, cols]), "wa": np.ascontiguousarray(w_in[:, :2048]),
            "modm": np.ascontiguousarray(np.stack([m[0], m[1]])), "g_mix": inp["norm_mix_g"][l],
            "cwT": np.ascontiguousarray(inp["conv_w"][l].T),
            "cvec": np.ascontiguousarray(np.stack([inp["conv_b"][l], inp["conv_ln_g"][l], inp["conv_ln_b"][l]])),
            "halo": np.full((128, 1), 0.0 if q == 0 else 1.0, np.float32),
            "hv": np.ascontiguousarray(np.stack([inp["fox_qn_g"][l], inp["fox_kn_g"][l], inp["fox_on_g"][l],
                                                 inp["dn_on_g"][l]])),
            "fb3": np.ascontiguousarray(inp["fox_f_bias"][l][hs].reshape(3, 1)),
            "dnp": dnp, "dcw": np.ascontiguousarray(np.concatenate(dcw_rows, axis=0)),
            "ident": ident, "ut": ut, "cmask": cmask,
        })
    res = run_bass_kernel_spmd(nc, in_maps, core_ids=list(range(8)))
    outs = res.results
    yT_full = np.zeros((8, D, TPC), ml_dtypes.bfloat16)
    for i in range(8):
        b, q = i // 4, i % 4
        yT_full[i, :CONV_CH] = outs[i]["yaT"]
        for qq in range(4):
            src = outs[b * 4 + qq]
            yT_full[i, CONV_CH + qq * 384: CONV_CH + (qq + 1) * 384] = src["ybT"][:, q * TPC:(q + 1) * TPC]
            yT_full[i, CONV_CH + FOX_W + qq * 384: CONV_CH + FOX_W + (qq + 1) * 384] = src["ycT"][:, q * TPC:(q + 1) * TPC]
    return yT_full, outs


def build_fused(n_exp=NE):
    P = Prog()
    IN, OUT, SCR = "ExternalInput", "ExternalOutput", "Internal"
    L = DEPTH
    d = {}
    c_in = P.dram("c", [1, D], F32, IN)
    aw = P.dram("ada_w", [L, D, 6 * 1024], F32, IN)
    ab = P.dram("ada_b", [L, 6 * 1024], F32, IN)
    sel = P.dram("sel", [128, 12], F32, IN)
    x_b = P.dram("x_b", [SEQ, D], F32, IN)
    x_c = P.dram("x_c", [CT_TOK, D], F32, IN)
    x_own = P.dram("x_own", [TPC, D], F32, IN)
    wc = P.dram("wc", [L, D, WC], F32, IN)
    wa = P.dram("wa", [L, D, 2048], F32, IN)
    gmix = P.dram("g_mix", [L, D], F32, IN)
    cwT = P.dram("cwT", [L, CONV_CH, 31], F32, IN)
    cvec = P.dram("cvec", [L, 3, CONV_CH], F32, IN)
    halo = P.dram("halo", [128, 1], F32, IN)
    hv = P.dram("hv", [L, 4, 128], F32, IN)
    fb3 = P.dram("fb3", [L, 3, 1], F32, IN)
    dnp = P.dram("dnp", [L, 2, 4], F32, IN)
    dcw = P.dram("dcw", [L, 9 * 128, 4], F32, IN)
    ident = P.dram("ident", [128, 128], F32, IN)
    ut = P.dram("ut", [128, 128], F32, IN)
    cmask = P.dram("cmask", [128, 4, 512], BF16, IN)
    wout = P.dram("w_out", [L, D, D], F32, IN)
    gffn = P.dram("g_ffn", [L, D], F32, IN)
    fing = P.dram("final_g", [D], F32, IN)
    wr = P.dram("w_router", [D, NE], F32, IN)
    rb = P.dram("router_bias", [NE], F32, IN)
    wg = P.dram("w_gate", [L, NE, D, DE], F32, IN)
    wu = P.dram("w_up", [L, NE, D, DE], F32, IN)
    wd = P.dram("w_down", [L, NE, DE, D], F32, IN)
    outn = P.dram("outn", [TPC, D], F32, OUT)
    mod_sh = P.dram("mod_sh", [L * 6, 1024], F32, SCR)
    mod_all = P.dram("mod_all", [4 * L * 6, 1024], F32, SCR)
    modm = P.dram("modm", [2, D], F32, SCR)
    modv = P.dram("modv", [4, D], F32, SCR)
    ycomb = P.dram("ycomb", [768, SEQ], BF16, SCR)
    ygat = P.dram("ygat", [6, 4 * 128, SEQ], BF16, SCR)
    yaT = P.dram("yaT", [CONV_CH, 1024], BF16, SCR)
    yT = P.dram("yT", [D, TPC], BF16, SCR)
    x2 = P.dram("x2", [TPC, D], F32, SCR)
    xg = P.dram("xg", [16, 256, D], F32, SCR)
    x_c1 = P.dram("x_c1", [CT_TOK, D], F32, SCR)
    scrA = dict(hTd=P.dram("hTd", [128, KC, SEQ], BF16, SCR), hTc=P.dram("hTc", [128, KC, CT_TOK], BF16, SCR),
                projT=P.dram("projT", [NFM, 128, SEQ], F32, SCR), ffd=P.dram("ffd", [3, SEQ], F32, SCR),
                Fd=P.dram("Fd", [3, SEQ], F32, SCR), projM=P.dram("projM", [SEQ, TMW], F32, SCR),
                aT=P.dram("aT", [16, 128, CT_TOK], F32, SCR))
    scrB = dict(x1=P.dram("x1", [TPC, D], F32, SCR), acc=P.dram("acc", [TPC, D], F32, SCR),
                hTdB=P.dram("hT_scr", [128, KC, TPC], BF16, SCR),
                gates_d=P.dram("gates_scr", [128, NTT, NE], F32, SCR))
    G8 = [list(range(8))]
    G4 = [[0, 1, 2, 3], [4, 5, 6, 7]]

    def lv(t, l, name):
        return Tile(t.t[l], f"{name}{l}")

    def pv(parent, ap):
        v = Tile(ap, parent.trk.name)
        v.trk = parent.trk
        return v

    MQ = 6 * 1024
    P.begin_phase()
    cT = P.sb([128, KC, 1], F32, "cT")
    P.dma(cT[:, :, 0], c_in.t[0].rearrange("(k p) -> p k", p=128), reads=[c_in], writes=[cT], semtrk=cT,
          allow_slow_non_contiguous=True)
    cA = P.sb([128, KC, 1], F32, "cA")
    P.act(cA[:], cT[:], AF.Silu, [cT], [cA])
    wb = [P.sb([128, KC, 512], F32, f"w{i}") for i in range(2)]
    pss = [P.ps([128, 512], F32, f"ps{i}") for i in range(2)]
    res = P.sb([1, MQ], F32, "res")
    bias = P.sb([1, MQ], F32, "bias")
    it = 0
    for l in range(L):
        P.dma(bias[:], ab.t[l:l + 1, :], reads=[ab], writes=[bias], semtrk=bias)
        for ct in range(MQ // 512):
            w = wb[it % 2]
            ps = pss[it % 2]
            src = aw.t[l].rearrange("(k p) n -> p k n", p=128)[:, :, ct * 512:(ct + 1) * 512]
            half = KC // 2
            P.dma_group([(w[:, :half, :], src[:, :half, :]), (w[:, half:, :], src[:, half:, :])],
                        reads=[aw], writes=[w], semtrk=w, q="sync")
            for k in range(KC):
                P.mm(ps[:1, :], cA[:, k, :], w[:, k, :], k == 0, k == KC - 1, [cA, w], [ps])
            P.tt(res[:, ct * 512:(ct + 1) * 512], ps[:1, :], bias[:, ct * 512:(ct + 1) * 512], ALU.add,
                 [ps, bias], [res])
            it += 1
        P.dma(mod_sh.t[l * 6:(l + 1) * 6, :].rearrange("(o s) j -> o (s j)", o=1), res[:], reads=[res],
              writes=[mod_sh], semtrk=res)
    P.coll("AllGather", mod_all, mod_sh, G4)
    P.end_phase()
    mav = mod_all.t.rearrange("(r l s) j -> l s r j", r=4, l=L, s=6)

    for l in range(L):
        P.begin_phase()
        selt = P.sb([128, 12], F32, "selt")
        P.dma(selt[:], sel.t, reads=[sel], writes=[selt], semtrk=selt)
        for si in range(6):
            dst = (modm.t[si] if si < 2 else modv.t[si - 2]).rearrange("(r j) -> r j", j=1024)
            P.dma(dst, mav[l, si], reads=[mod_all], writes=[modm if si < 2 else modv], semtrk=Trk("d2d"))
        if l > 0:
            for c in range(16):
                P.coll("AllGather", pv(xg, xg.t[c]), pv(x2, x2.t[c * 64:(c + 1) * 64, :]), G4)
            P.dma(x_c1.t[128:, :], x2.t, reads=[x2], writes=[x_c1.sub("own")], semtrk=Trk("d2d"))
            h0 = P.sb([128, D], F32, "h0")
            cands = [P.sb([128, D], F32, f"cand{i}") for i in range(2)]
            for r in range(3):
                cd = cands[r % 2]
                P.dma_group([(cd[0:64, :], xg.t[14, r * 64:(r + 1) * 64, :]), (cd[64:128, :], xg.t[15, r * 64:(r + 1) * 64, :])],
                            reads=[xg], writes=[cd], semtrk=cd)
                if r == 0:
                    P.ts(h0[:], cd[:], selt[:, 6:7], None, ALU.mult, None, [cd, selt], [h0])
                else:
                    P.stt(h0[:], cd[:], selt[:, 6 + r:7 + r], h0[:], ALU.mult, ALU.add, [cd, selt, h0], [h0])
            P.dma(x_c1.t[0:128, :], h0[:], reads=[h0], writes=[x_c1.sub("halo")], semtrk=h0)
        P.end_phase()

        def xg_rows(tt):
            r, t8 = tt // 8, tt % 8
            return [(0, 64, xg.t[2 * t8, r * 64:(r + 1) * 64, :]), (64, 128, xg.t[2 * t8 + 1, r * 64:(r + 1) * 64, :])]

        ctxA = dict(P=P, x_b_rows=None if l == 0 else xg_rows, x_b=x_b if l == 0 else xg, x_c=x_c if l == 0 else x_c1, wc=lv(wc, l, "wc"),
                    wa=lv(wa, l, "wa"), modm=modm, gmix=lv(gmix, l, "gmix"), cwT=lv(cwT, l, "cwT"),
                    cvec=lv(cvec, l, "cvec"), halo=halo, hv=lv(hv, l, "hv"), fb3=lv(fb3, l, "fb3"),
                    dnp=lv(dnp, l, "dnp"), dcw=lv(dcw, l, "dcw"), ident=ident, ut=ut, cmask=cmask,
                    ybT=Tile(ycomb.t[0:384], "ybT"), ycT=Tile(ycomb.t[384:768], "ycT"), yaT=yaT, **scrA)
        build_A(ctx=ctxA)

        P.begin_phase()
        for p in range(6):
            P.coll("AllGather", pv(ygat, ygat.t[p]), pv(ycomb, ycomb.t[p * 128:(p + 1) * 128, :]), G4)
        P.dma(yT.t[0:CONV_CH, :], yaT.t, reads=[yaT], writes=[yT.sub("a")], semtrk=Trk("d2d"))
        selt = P.sb([128, 12], F32, "selt")
        P.dma(selt[:], sel.t, reads=[sel], writes=[selt], semtrk=selt)
        cnds = [P.sb([128, 4, TPC], BF16, f"cnd{i}") for i in range(3)]
        accs = [P.sb([128, TPC], BF16, f"yacc{i}") for i in range(3)]
        for ch in range(24):
            hd_ = ch % 12
            r, hh = hd_ // 3, hd_ % 3
            piece = hh if ch < 12 else 3 + hh
            cd = cnds[ch % 3]
            ac = accs[ch % 3]
            P.dma(cd[:], ygat.t[piece, r * 128:(r + 1) * 128, :].rearrange("p (q t) -> p q t", q=4), reads=[ygat],
                  writes=[cd], semtrk=cd)
            P.ts(ac[:], cd[:, 0, :], selt[:, 0:1], None, ALU.mult, None, [cd, selt], [ac])
            for qq in range(1, 4):
                P.stt(ac[:], cd[:, qq, :], selt[:, qq:qq + 1], ac[:], ALU.mult, ALU.add, [cd, selt, ac], [ac])
            P.dma(yT.t[CONV_CH + ch * 128:CONV_CH + (ch + 1) * 128, :], ac[:], reads=[ac], writes=[yT.sub(ch)],
                  semtrk=ac)
        P.end_phase()

        last = l == L - 1
        ctxB = dict(P=P, x=x_own if l == 0 else x2, yT=yT, wout=lv(wout, l, "wout"), modv=modv,
                    gffn=lv(gffn, l, "gffn"), fing=fing, wr=wr, rb=rb, wg=lv(wg, l, "wg"), wu=lv(wu, l, "wu"),
                    wd=lv(wd, l, "wd"), ident=ident, out=None if last else x2, outn=outn if last else None, **scrB)
        build_B(n_exp, ctx=ctxB)
    return P.finish()


def fused_inputs(inp):
    ident, ut, cmask = host_consts()
    x3 = inp["x"]
    L = DEPTH
    in_maps = []
    for i in range(8):
        b, q = i // 4, i % 4
        hs = [3 * q + hh for hh in range(3)]
        mcols = np.concatenate([np.arange(s * D + q * 1024, s * D + (q + 1) * 1024) for s in range(6)])
        cols = []
        for base in (O_FQ, O_FK, O_FG, O_DQ, O_DK, O_DV):
            for hh in hs:
                cols.append(np.arange(base + hh * 128, base + (hh + 1) * 128))
        cols.append(np.array([O_FF + hh for hh in hs]))
        for base in (O_FV, O_DZ):
            for hh in hs:
                cols.append(np.arange(base + hh * 128, base + (hh + 1) * 128))
        cols.append(np.array([O_DB + hh for hh in hs]))
        cols.append(np.array([O_DA + hh for hh in hs]))
        cols = np.concatenate(cols)
        xc = np.zeros((CT_TOK, D), np.float32)
        if q == 0:
            xc[128:] = x3[b, :1024]
        else:
            xc[:] = x3[b, q * 1024 - 128:(q + 1) * 1024]
        selv = np.zeros((12,), np.float32)
        selv[q] = 1.0
        selv[4 + b] = 1.0
        if q > 0:
            selv[6 + q - 1] = 1.0
        dcw_l, dnp_l = [], []
        for l in range(L):
            rows = []
            for comp in range(3):
                for hh in hs:
                    rows.append(inp["dn_conv_w"][l][:, comp * DN_W + hh * 128: comp * DN_W + (hh + 1) * 128].T)
            dcw_l.append(np.concatenate(rows, axis=0))
            dn = np.zeros((2, 4), np.float32)
            dn[0, :3] = inp["dn_a_log"][l][hs]
            dn[1, :3] = inp["dn_dt_bias"][l][hs]
            dnp_l.append(dn)
        in_maps.append({
            "c": np.ascontiguousarray(inp["c"][b:b + 1]),
            "ada_w": np.ascontiguousarray(inp["ada_w"][:, :, mcols]),
            "ada_b": np.ascontiguousarray(inp["ada_b"][:, mcols]),
            "sel": np.ascontiguousarray(np.broadcast_to(selv, (128, 12))),
            "x_b": np.ascontiguousarray(x3[b]), "x_c": xc,
            "x_own": np.ascontiguousarray(x3[b, q * TPC:(q + 1) * TPC]),
            "wc": np.ascontiguousarray(inp["w_in"][:, :, cols]),
            "wa": np.ascontiguousarray(inp["w_in"][:, :, :2048]),
            "g_mix": inp["norm_mix_g"],
            "cwT": np.ascontiguousarray(inp["conv_w"].transpose(0, 2, 1)),
            "cvec": np.ascontiguousarray(np.stack([inp["conv_b"], inp["conv_ln_g"], inp["conv_ln_b"]], axis=1)),
            "halo": np.full((128, 1), 0.0 if q == 0 else 1.0, np.float32),
            "hv": np.ascontiguousarray(np.stack([inp["fox_qn_g"], inp["fox_kn_g"], inp["fox_on_g"],
                                                 inp["dn_on_g"]], axis=1)),
            "fb3": np.ascontiguousarray(inp["fox_f_bias"][:, hs].reshape(L, 3, 1)),
            "dnp": np.stack(dnp_l), "dcw": np.ascontiguousarray(np.stack(dcw_l)),
            "ident": ident, "ut": ut, "cmask": cmask,
            "w_out": inp["w_out"], "g_ffn": inp["norm_ffn_g"], "final_g": inp["final_g"],
            "w_router": inp["w_router"], "router_bias": inp["router_bias"],
            "w_gate": inp["w_gate_e"], "w_up": inp["w_up_e"], "w_down": inp["w_down_e"],
        })
    return in_maps


def kernel(**inp):
    inp = {k: np.ascontiguousarray(np.asarray(v), dtype=np.float32) for k, v in inp.items()}
    nc = build_fused()
    in_maps = fused_inputs(inp)
    res = run_bass_kernel_spmd(nc, in_maps, core_ids=list(range(8)))
    out = np.concatenate([r["outn"] for r in res.results], axis=0)
    return out.reshape(NB, SEQ, D)
```

```python
import contextlib
import numpy as np
import ml_dtypes
import concourse.bass as bass
import concourse.mybir as mybir
from concourse.bass_utils import run_bass_kernel_spmd

F32 = mybir.dt.float32
BF16 = mybir.dt.bfloat16
AF = mybir.ActivationFunctionType
ALU = mybir.AluOpType
AX = mybir.AxisListType

D = 4096
SEQ = 4096
NB = 2
DEPTH = 2
HD = 128
CONV_CH = 1024
NH = 12
FOX_W = 1536
DN_W = 1536
N_IN = 14372
NE = 16
DE = 1024
EPS = 1e-6
KC = D // 128

O_AVAL = 0
O_AGATE = 1024
O_FQ = 2048
O_FK = O_FQ + FOX_W
O_FV = O_FK + FOX_W
O_FF = O_FV + FOX_W
O_FG = O_FF + NH
O_DQ = O_FG + FOX_W
O_DK = O_DQ + DN_W
O_DV = O_DK + DN_W
O_DB = O_DV + DN_W
O_DA = O_DB + NH
O_DZ = O_DA + NH
assert O_DZ + DN_W == N_IN


class Trk:
    __slots__ = ("w", "r", "sem", "semcnt", "name", "excl")

    def __init__(self, name=""):
        self.excl = False
        self.w = {}
        self.r = {}
        self.sem = None
        self.semcnt = 0
        self.name = name


class Tile:
    def __init__(self, t, name):
        self.t = t
        self.trk = Trk(name)
        self.subs = {}

    def __getitem__(self, idx):
        return self.t[idx]

    def sub(self, key):
        if key not in self.subs:
            self.subs[key] = Trk(f"{self.trk.name}.{key}")
        return self.subs[key]


def _trk(x):
    return x.trk if isinstance(x, Tile) else x


import os as _os
SAME_ENGINE_SYNC = _os.environ.get("K_SES", "1") == "1"


class Prog:
    ENGS = ("tensor", "vector", "scalar", "gpsimd", "sync")

    def __init__(self, same_engine_sync=SAME_ENGINE_SYNC):
        self.nc = bass.Bass("TRN2", target_bir_lowering=False)
        self.stack = contextlib.ExitStack()
        self.same_engine_sync = same_engine_sync
        self.esem = {}
        self.ecnt = {}
        self.known = {e: {} for e in self.ENGS}
        self.ops = {e: [] for e in self.ENGS}
        self.sems = {}
        for e in ("tensor", "vector", "scalar", "gpsimd"):
            s = self.stack.enter_context(self.nc.semaphore(f"e_{e}"))
            self.esem[e] = s
            self.sems[id(s)] = s
            self.ecnt[e] = 0
        self.free_dsems = []
        self.ndsem = 0
        self.phase_stack = None
        self.uid = 0
        self.all_dma_trk = []
        self.phase_trks = []
        self.dsem_latest = {}

    def dram(self, name, shape, dt, kind, **kw):
        t = self.nc.dram_tensor(name, list(shape), dt, kind=kind, **kw).ap()
        return Tile(t, name)

    def begin_phase(self):
        self.phase_stack = contextlib.ExitStack()

    def sb(self, shape, dt, name=None):
        self.uid += 1
        name = name or f"t{self.uid}"
        t = self.phase_stack.enter_context(self.nc.sbuf_tensor(f"{name}_{self.uid}", list(shape), dt))
        return Tile(t, name)

    def ps(self, shape, dt=F32, name=None):
        self.uid += 1
        name = name or f"p{self.uid}"
        t = self.phase_stack.enter_context(self.nc.psum_tensor(f"{name}_{self.uid}", list(shape), dt))
        tl = Tile(t, name)
        tl.trk.excl = True
        return tl

    def _dsem(self, trk):
        if trk.sem is None:
            if self.free_dsems:
                s = self.free_dsems.pop()
                trk.semcnt = self.dsem_latest.get(id(s), 0)
            else:
                self.ndsem += 1
                s = self.stack.enter_context(self.nc.semaphore(f"d{self.ndsem}"))
                self.sems[id(s)] = s
            trk.sem = s
            self.phase_trks.append(trk)
        return trk.sem

    def _collect(self, eng, reads, writes):
        need = {}
        for b in reads:
            for k, v in _trk(b).w.items():
                if need.get(k, 0) < v:
                    need[k] = v
        for b in writes:
            tb = _trk(b)
            for dct in (tb.w, tb.r):
                for k, v in dct.items():
                    if need.get(k, 0) < v:
                        need[k] = v
        waits = []
        kn = self.known[eng]
        own = id(self.esem[eng]) if eng in self.esem else None
        for k, v in need.items():
            if k == own and (eng == "tensor" or not self.same_engine_sync):
                continue
            if kn.get(k, 0) >= v:
                continue
            kn[k] = v
            waits.append((self.sems[k], v))
        return waits

    def _mark(self, ev, reads, writes):
        k, v = ev
        for b in reads:
            tb = _trk(b)
            if tb.r.get(k, 0) < v:
                tb.r[k] = v
        for b in writes:
            tb = _trk(b)
            tb.w = {k: v}
            tb.r = {}

    def op(self, eng, fn, reads=(), writes=()):
        ex = [b for b in reads if _trk(b).excl]
        if ex:
            reads = [b for b in reads if not _trk(b).excl]
            writes = list(writes) + ex
        waits = self._collect(eng, reads, writes)
        self.ecnt[eng] += 1
        sem = self.esem[eng]
        self.ops[eng].append((waits, fn, sem, 1))
        self._mark((id(sem), self.ecnt[eng]), reads, writes)

    def dma(self, out_ap, in_ap, reads=(), writes=(), semtrk=None, q="sync", **kw):
        self.dma_group([(out_ap, in_ap)], reads, writes, semtrk, q, **kw)

    def dma_group(self, pairs, reads=(), writes=(), semtrk=None, q="sync", **kw):
        semtrk = _trk(semtrk)
        sem = self._dsem(semtrk)
        waits = self._collect(q, reads, writes)
        kn = self.known[q]
        if semtrk.semcnt and kn.get(id(sem), 0) < semtrk.semcnt:
            kn[id(sem)] = semtrk.semcnt
            waits = [w for w in waits if w[0] is not sem] + [(sem, semtrk.semcnt)]
        for i, (out_ap, in_ap) in enumerate(pairs):
            semtrk.semcnt += 16

            def fn(e, out_ap=out_ap, in_ap=in_ap, kw=kw):
                return e.dma_start(out=out_ap, in_=in_ap, **kw)

            self.ops[q].append((waits if i == 0 else [], fn, sem, 16))
        self.dsem_latest[id(sem)] = semtrk.semcnt
        self._mark((id(sem), semtrk.semcnt), reads, writes)

    def coll(self, kind, out_tile, in_tile, groups, op=None, inc=1):
        ctrk = Trk("coll")
        sem = self._dsem(ctrk)
        waits = self._collect("gpsimd", [in_tile], [out_tile])
        kn = self.known["gpsimd"]
        if ctrk.semcnt and kn.get(id(sem), 0) < ctrk.semcnt:
            kn[id(sem)] = ctrk.semcnt
            waits = [w for w in waits if w[0] is not sem] + [(sem, ctrk.semcnt)]
        ctrk.semcnt += inc
        op = op if op is not None else ALU.bypass

        def fn(e):
            return e.collective_compute(kind, op, groups, [in_tile.t.opt()], [out_tile.t.opt()])

        self.ops["gpsimd"].append((waits, fn, sem, inc))
        self.dsem_latest[id(sem)] = ctrk.semcnt
        self._mark((id(sem), ctrk.semcnt), [in_tile], [out_tile])

    def barrier_all(self):
        tgt = {}
        for e, s in self.esem.items():
            if self.ecnt[e]:
                tgt[id(s)] = self.ecnt[e]
        for k, v in self.dsem_latest.items():
            if v:
                tgt[k] = v
        for e in self.ENGS:
            waits = []
            kn = self.known[e]
            for k, v in tgt.items():
                if kn.get(k, 0) < v:
                    kn[k] = v
                    waits.append((self.sems[k], v))
            if waits:
                self.ops[e].append((waits, None, None, 0))

    def end_phase(self):
        self.barrier_all()
        nc = self.nc
        ops = self.ops
        with nc.Block() as block:
            def mk(ename):
                def body(e):
                    for waits, fn, sem, amt in ops[ename]:
                        for s, v in waits:
                            e.wait_ge(s, v)
                        if fn is not None:
                            fn(e).then_inc(sem, amt)
                return body
            block.tensor(mk("tensor"))
            block.vector(mk("vector"))
            block.scalar(mk("scalar"))
            block.gpsimd(mk("gpsimd"))
            block.sync(mk("sync"))
        self.ops = {e: [] for e in self.ENGS}
        self.phase_stack.close()
        self.phase_stack = None
        for t in self.phase_trks:
            self.free_dsems.append(t.sem)
            t.sem = None
        self.phase_trks = []

    def finish(self):
        self.stack.close()
        return self.nc

    def mm(self, out, lhsT, rhs, start, stop, reads, writes):
        self.op("tensor", lambda e: e.matmul(out, lhsT, rhs, start=start, stop=stop), reads, writes)

    def act(self, out, in_, func, reads, writes, bias=None, scale=None, accum_out=None, eng="scalar"):
        kw = {}
        if bias is not None:
            kw["bias"] = bias
        if scale is not None:
            kw["scale"] = scale
        if accum_out is not None:
            kw["accum_out"] = accum_out
        self.op(eng, lambda e: e.activation(out, in_, func, **kw), reads, writes)

    def ts(self, out, in0, s1, s2, op0, op1, reads, writes, eng="vector", accum_out=None):
        kw = {}
        if accum_out is not None:
            kw["accum_out"] = accum_out
        if op1 is None:
            self.op(eng, lambda e: e.tensor_scalar(out, in0, s1, None, op0, **kw), reads, writes)
        else:
            self.op(eng, lambda e: e.tensor_scalar(out, in0, s1, s2, op0, op1, **kw), reads, writes)

    def tt(self, out, in0, in1, op, reads, writes, eng="vector"):
        self.op(eng, lambda e: e.tensor_tensor(out, in0, in1, op), reads, writes)

    def stt(self, out, in0, scalar, in1, op0, op1, reads, writes, eng="vector"):
        self.op(eng, lambda e: e.scalar_tensor_tensor(out, in0, scalar, in1, op0, op1), reads, writes)

    def copy(self, out, in_, reads, writes, eng="vector"):
        if eng == "scalar":
            self.op(eng, lambda e: e.activation(out, in_, AF.Copy), reads, writes)
        else:
            self.op(eng, lambda e: e.tensor_copy(out, in_), reads, writes)

    def memset(self, ap, val, writes, eng="vector"):
        self.op(eng, lambda e: e.memset(ap, val), (), writes)


MCOLS = 6 * D // 8


def build_mod():
    P = Prog()
    nc = P.nc
    c_in = P.dram("c", [NB, D], F32, "ExternalInput")
    aw = P.dram("ada_w", [DEPTH, D, MCOLS], F32, "ExternalInput")
    ab = P.dram("ada_b", [DEPTH, MCOLS], F32, "ExternalInput")
    out = P.dram("mod", [DEPTH, NB, MCOLS], F32, "ExternalOutput")
    P.begin_phase()
    cT = P.sb([128, KC, NB], F32, "cT")
    P.dma_group([(cT[:, :, b], c_in.t[b].rearrange("(k p) -> p k", p=128)) for b in range(NB)],
                reads=[c_in], writes=[cT], semtrk=cT, allow_slow_non_contiguous=True)
    cA = P.sb([128, KC, NB], F32, "cA")
    P.act(cA[:], cT[:], AF.Silu, [cT], [cA])
    wb = [P.sb([128, KC, 512], F32, f"w{i}") for i in range(2)]
    pss = [P.ps([128, 512], F32, f"ps{i}") for i in range(2)]
    res = P.sb([NB, DEPTH, MCOLS], F32, "res")
    bias = P.sb([NB, DEPTH, MCOLS], F32, "bias")
    for b in range(NB):
        P.dma(bias[b:b + 1, :, :], ab.t.rearrange("(o l) n -> o l n", o=1), reads=[ab], writes=[bias], semtrk=bias)
    it = 0
    for l in range(DEPTH):
        for ct in range(MCOLS // 512):
            w = wb[it % 2]
            ps = pss[it % 2]
            src = aw.t[l].rearrange("(k p) n -> p k n", p=128)[:, :, ct * 512:(ct + 1) * 512]
            half = KC // 2
            P.dma_group([(w[:, :half, :], src[:, :half, :]), (w[:, half:, :], src[:, half:, :])],
                        reads=[aw], writes=[w], semtrk=w, q="sync")
            for k in range(KC):
                P.mm(ps[:NB, :], cA[:, k, :], w[:, k, :], k == 0, k == KC - 1, [cA, w], [ps])
            P.tt(res[:, l, ct * 512:(ct + 1) * 512], ps[:NB, :], bias[:, l, ct * 512:(ct + 1) * 512], ALU.add,
                 [ps, bias], [res])
            it += 1
    P.dma(out.t.rearrange("l b n -> b l n"), res[:], reads=[res], writes=[out], semtrk=res)
    P.end_phase()
    return P.finish()


def run_mod(c, ada_w, ada_b):
    nc = build_mod()
    in_maps = []
    for i in range(8):
        sl = slice(i * MCOLS, (i + 1) * MCOLS)
        in_maps.append({"c": np.ascontiguousarray(c),
                        "ada_w": np.ascontiguousarray(ada_w[:, :, sl]),
                        "ada_b": np.ascontiguousarray(ada_b[:, sl])})
    res = run_bass_kernel_spmd(nc, in_maps, core_ids=list(range(8)))
    return np.concatenate([r["mod"] for r in res.results], axis=2)


TPC = 1024
NTT = TPC // 128


def build_B(n_exp=NE, ctx=None):
    if ctx is not None:
        P = ctx["P"]
        nc = P.nc
        (x, yT, wout, modv, gffn, fing, wr, rb, wg, wu, wd, ident_d, out, outn, x1, acc, hTd, gates_d) = (
            ctx[k] for k in ("x", "yT", "wout", "modv", "gffn", "fing", "wr", "rb", "wg", "wu", "wd", "ident",
                             "out", "outn", "x1", "acc", "hTdB", "gates_d"))
    else:
        P = Prog()
        nc = P.nc
        x = P.dram("x", [TPC, D], F32, "ExternalInput")
        yT = P.dram("yT", [D, TPC], BF16, "ExternalInput")
        wout = P.dram("w_out", [D, D], F32, "ExternalInput")
        modv = P.dram("modv", [4, D], F32, "ExternalInput")
        gffn = P.dram("g_ffn", [D], F32, "ExternalInput")
        fing = P.dram("final_g", [D], F32, "ExternalInput")
        wr = P.dram("w_router", [D, NE], F32, "ExternalInput")
        rb = P.dram("router_bias", [NE], F32, "ExternalInput")
        wg = P.dram("w_gate", [NE, D, DE], F32, "ExternalInput")
        wu = P.dram("w_up", [NE, D, DE], F32, "ExternalInput")
        wd = P.dram("w_down", [NE, DE, D], F32, "ExternalInput")
        ident_d = P.dram("ident", [128, 128], F32, "ExternalInput")
        out = P.dram("out", [TPC, D], F32, "ExternalOutput")
        outn = P.dram("outn", [TPC, D], F32, "ExternalOutput")
        x1 = P.dram("x1", [TPC, D], F32, "Internal")
        acc = P.dram("acc", [TPC, D], F32, "Internal")
        hTd = P.dram("hT_scr", [128, KC, TPC], BF16, "Internal")
        gates_d = P.dram("gates_scr", [128, NTT, NE], F32, "Internal")

    P.begin_phase()
    yTs = P.sb([128, KC, TPC], BF16, "yTs")
    P.dma_group([(yTs[:, :16, :], yT.t.rearrange("(k p) t -> p k t", p=128)[:, :16, :]),
                 (yTs[:, 16:, :], yT.t.rearrange("(k p) t -> p k t", p=128)[:, 16:, :])],
                reads=[yT], writes=[yTs], semtrk=yTs)
    gm = P.sb([128, D], F32, "gm")
    P.dma(gm[:], modv.t[0:1, :].partition_broadcast(128), reads=[modv], writes=[gm], semtrk=gm)
    wbs = [P.sb([128, KC, 512], BF16, f"wb{i}") for i in range(2)]
    xios = [P.sb([128, 512], F32, f"xio{i}") for i in range(4)]
    tmps = [P.sb([128, 512], F32, f"tmp{i}") for i in range(2)]
    pss = [P.ps([128, 512], F32, f"ps{i}") for i in range(4)]
    woutv = wout.t.rearrange("(k p) n -> p k n", p=128)
    it = 0
    for ct in range(D // 512):
        cs = slice(ct * 512, (ct + 1) * 512)
        wb = wbs[ct % 2]
        P.dma_group([(wb[:, :16, :], woutv[:, :16, cs]), (wb[:, 16:, :], woutv[:, 16:, cs])],
                    reads=[wout], writes=[wb], semtrk=wb, q="gpsimd")
        for tt in range(NTT):
            ps = pss[it % 4]
            xio = xios[it % 4]
            tmp = tmps[it % 2]
            rs = slice(tt * 128, (tt + 1) * 128)
            P.dma(xio[:], x.t[rs, cs], reads=[x], writes=[xio], semtrk=xio)
            for k in range(KC):
                P.mm(ps[:], yTs[:, k, rs], wb[:, k, :], k == 0, k == KC - 1, [yTs, wb], [ps])
            P.tt(tmp[:], ps[:], gm[:, cs], ALU.mult, [ps, gm], [tmp])
            P.tt(xio[:], tmp[:], xio[:], ALU.add, [tmp, xio], [xio], eng="gpsimd")
            P.dma(x1.t[rs, cs], xio[:], reads=[xio], writes=[x1.sub(tt)], semtrk=xio)
            it += 1
    P.end_phase()

    P.begin_phase()
    ident = P.sb([128, 128], F32, "ident")
    P.dma(ident[:], ident_d.t, reads=[ident_d], writes=[ident], semtrk=ident)
    gT = P.sb([128, KC], F32, "gT")
    scT = P.sb([128, KC], F32, "scT")
    BvT = P.sb([128, KC], F32, "BvT")
    AT = P.sb([128, KC], F32, "AT")
    P.dma(gT[:], gffn.t.rearrange("(k p) -> p k", p=128), reads=[gffn], writes=[gT], semtrk=gT,
          allow_slow_non_contiguous=True)
    P.dma(scT[:], modv.t[2].rearrange("(k p) -> p k", p=128), reads=[modv], writes=[scT], semtrk=scT,
          allow_slow_non_contiguous=True)
    P.dma(BvT[:], modv.t[1].rearrange("(k p) -> p k", p=128), reads=[modv], writes=[BvT], semtrk=BvT,
          allow_slow_non_contiguous=True)
    P.stt(AT[:], scT[:], 1.0, gT[:], ALU.add, ALU.mult, [scT, gT], [AT])
    wrs = P.sb([128, KC, NE], F32, "wrs")
    P.dma(wrs[:], wr.t.rearrange("(k p) e -> p k e", p=128), reads=[wr], writes=[wrs], semtrk=wrs)
    rbb = P.sb([128, NE], F32, "rbb")
    P.dma(rbb[:], rb.t.rearrange("(o e) -> o e", o=1).partition_broadcast(128), reads=[rb], writes=[rbb],
          semtrk=rbb)
    xrs = [P.sb([128, D], F32, f"xr{i}") for i in range(2)]
    junk = P.sb([128, D], BF16, "junk")
    xs = P.sb([128, D], F32, "xs")
    h32 = P.sb([128, KC, 128], F32, "h32")
    hTt = [P.sb([128, KC, 128], BF16, f"hTt{i}") for i in range(2)]
    gates = P.sb([128, NTT, NE], F32, "gates")
    sm = {n: P.sb([128, 1], F32, n) for n in ("ss", "ms", "rstd", "gmax", "den", "rden")}
    r16 = {n: P.sb([128, NE], F32, n) for n in ("sc", "sel", "eq", "sel2", "in2", "selm", "gsel")}
    r4 = {n: P.sb([128, 4], F32, n) for n in ("m1", "m2", "gs", "gmask")}
    pst = [P.ps([128, 512], F32, f"pst{i}") for i in range(4)]
    psr = P.ps([128, NE], F32, "psr")

    def v3(t):
        return t[:].rearrange("p (g e) -> p g e", g=4)

    def b3(t):
        return t[:].unsqueeze(2).to_broadcast([128, 4, 4])

    for tt in range(NTT):
        xr = xrs[tt % 2]
        rs = slice(tt * 128, (tt + 1) * 128)
        P.dma(xr[:], x1.t[rs, :], reads=[x1.sub(tt)], writes=[xr], semtrk=xr)
        P.act(junk[:], xr[:], AF.Square, [xr], [junk, sm["ss"]], accum_out=sm["ss"][:])
        P.ts(sm["ms"][:], sm["ss"][:], 1.0 / D, EPS, ALU.mult, ALU.add, [sm["ss"]], [sm["ms"]])
        P.act(sm["ms"][:], sm["ms"][:], AF.Sqrt, [sm["ms"]], [sm["ms"]])
        P.op("vector", lambda e: e.reciprocal(sm["rstd"][:], sm["ms"][:]), [sm["ms"]], [sm["rstd"]])
        P.act(xs[:], xr[:], AF.Identity, [xr, sm["rstd"]], [xs], scale=sm["rstd"][:])
        for kq in range(KC // 4):
            ps = pst[kq % 4]
            for j in range(4):
                k = kq * 4 + j
                P.op("tensor", lambda e, ps=ps, j=j, k=k: e.transpose(ps[:, j * 128:(j + 1) * 128],
                                                                     xs[:, k * 128:(k + 1) * 128], ident[:]),
                     [xs, ident], [ps])
            for j in range(4):
                k = kq * 4 + j
                P.ts(h32[:, k, :], ps[:, j * 128:(j + 1) * 128], AT[:, k:k + 1], BvT[:, k:k + 1], ALU.mult,
                     ALU.add, [ps, AT, BvT], [h32], eng="vector")
        hb = hTt[tt % 2]
        P.act(hb[:], h32[:], AF.Copy, [h32], [hb])
        P.dma(hTd.t[:, :, rs], hb[:], reads=[hb], writes=[hTd.sub(tt)], semtrk=hb)
        for k in range(KC):
            P.mm(psr[:], h32[:, k, :], wrs[:, k, :], k == 0, k == KC - 1, [h32, wrs], [psr])
        sc, sel, eq, sel2, in2, selm, gsel = (r16[n] for n in ("sc", "sel", "eq", "sel2", "in2", "selm", "gsel"))
        m1, m2, gs, gmask = (r4[n] for n in ("m1", "m2", "gs", "gmask"))
        P.act(sc[:], psr[:], AF.Sigmoid, [psr], [sc])
        P.tt(sel[:], sc[:], rbb[:], ALU.add, [sc, rbb], [sel])
        P.op("vector", lambda e, sel=sel, m1=m1: e.tensor_reduce(m1[:], v3(sel), AX.X, ALU.max), [sel], [m1])
        P.tt(v3(eq), v3(sel), b3(m1), ALU.is_equal, [sel, m1], [eq])
        P.stt(sel2[:], eq[:], -1e30, sel[:], ALU.mult, ALU.add, [eq, sel], [sel2])
        P.op("vector", lambda e, sel2=sel2, m2=m2: e.tensor_reduce(m2[:], v3(sel2), AX.X, ALU.max), [sel2], [m2])
        P.tt(gs[:], m1[:], m2[:], ALU.add, [m1, m2], [gs])
        P.op("vector", lambda e, gs=gs: e.tensor_reduce(sm["gmax"][:], gs[:], AX.X, ALU.max), [gs], [sm["gmax"]])
        P.tt(gmask[:], gs[:], sm["gmax"][:].to_broadcast([128, 4]), ALU.is_ge, [gs, sm["gmax"]], [gmask])
        P.tt(v3(in2), v3(sel), b3(m2), ALU.is_ge, [sel, m2], [in2])
        P.tt(v3(selm), v3(in2), b3(gmask), ALU.mult, [in2, gmask], [selm])
        P.tt(gsel[:], selm[:], sc[:], ALU.mult, [selm, sc], [gsel])
        P.op("vector", lambda e, gsel=gsel: e.tensor_reduce(sm["den"][:], gsel[:], AX.X, ALU.add), [gsel],
             [sm["den"]])
        P.op("vector", lambda e: e.reciprocal(sm["rden"][:], sm["den"][:]), [sm["den"]], [sm["rden"]])
        P.ts(gates[:, tt, :], gsel[:], sm["rden"][:], None, ALU.mult, None, [gsel, sm["rden"]], [gates])
    P.dma(gates_d.t, gates[:], reads=[gates], writes=[gates_d], semtrk=gates)
    P.end_phase()

    P.begin_phase()
    hT = P.sb([128, KC, TPC], BF16, "hT")
    P.dma_group([(hT[:, :, tt * 128:(tt + 1) * 128], hTd.t[:, :, tt * 128:(tt + 1) * 128]) for tt in range(NTT)],
                reads=[hTd.sub(tt) for tt in range(NTT)], writes=[hT], semtrk=hT)
    gates = P.sb([128, NTT, NE], F32, "gates")
    P.dma(gates[:], gates_d.t, reads=[gates_d], writes=[gates], semtrk=gates)
    wgb = [P.sb([128, KC, 128], BF16, f"wgb{i}") for i in range(2)]
    wub = [P.sb([128, KC, 128], BF16, f"wub{i}") for i in range(2)]
    wdb = [P.sb([128, 8, 512], BF16, f"wdb{i}") for i in range(2)]
    hid = [P.sb([128, 8, TPC], BF16, f"hid{i}") for i in range(2)]
    sgs = [P.sb([128, 512], F32, f"sg{i}") for i in range(2)]
    accio = [P.sb([128, 512], F32, f"accio{i}") for i in range(4)]
    psg = [P.ps([128, 512], F32, f"psg{i}") for i in range(2)]
    psu = [P.ps([128, 512], F32, f"psu{i}") for i in range(2)]
    psd = [P.ps([128, 512], F32, f"psd{i}") for i in range(4)]
    i1 = 0
    i2 = 0
    for e in range(n_exp):
        hd_ = hid[e % 2]
        wgv = wg.t[e].rearrange("(k p) n -> p k n", p=128)
        wuv = wu.t[e].rearrange("(k p) n -> p k n", p=128)
        for hc in range(8):
            wgt = wgb[hc % 2]
            wut = wub[hc % 2]
            hs = slice(hc * 128, (hc + 1) * 128)
            P.dma(wgt[:], wgv[:, :, hs], reads=[wg], writes=[wgt], semtrk=wgt, q="gpsimd")
            P.dma(wut[:], wuv[:, :, hs], reads=[wu], writes=[wut], semtrk=wut, q="gpsimd")
            for th in range(2):
                pg = psg[i1 % 2]
                pu = psu[i1 % 2]
                sg = sgs[i1 % 2]
                tsl = slice(th * 512, (th + 1) * 512)
                for k in range(KC):
                    P.mm(pg[:], wgt[:, k, :], hT[:, k, tsl], k == 0, k == KC - 1, [wgt, hT], [pg])
                for k in range(KC):
                    P.mm(pu[:], wut[:, k, :], hT[:, k, tsl], k == 0, k == KC - 1, [wut, hT], [pu])
                P.act(sg[:], pg[:], AF.Silu, [pg], [sg])
                P.tt(hd_[:, hc, tsl], sg[:], pu[:], ALU.mult, [sg, pu], [hd_])
                i1 += 1
        wdv = wd.t[e].rearrange("(c p) n -> p c n", p=128)
        for ct in range(D // 512):
            cs = slice(ct * 512, (ct + 1) * 512)
            wdt = wdb[ct % 2]
            P.dma(wdt[:], wdv[:, :, cs], reads=[wd], writes=[wdt], semtrk=wdt, q="gpsimd")
            for tt in range(NTT):
                rs = slice(tt * 128, (tt + 1) * 128)
                pd = psd[i2 % 4]
                ai = accio[i2 % 4]
                key = (tt, ct)
                if e > 0:
                    P.dma(ai[:], acc.t[rs, cs], reads=[acc.sub(key)], writes=[ai], semtrk=ai)
                for c in range(8):
                    P.mm(pd[:], hd_[:, c, rs], wdt[:, c, :], c == 0, c == 7, [hd_, wdt], [pd])
                if e > 0:
                    P.stt(ai[:], pd[:], gates[:, tt, e:e + 1], ai[:], ALU.mult, ALU.add, [pd, gates, ai], [ai])
                else:
                    P.ts(ai[:], pd[:], gates[:, tt, e:e + 1], None, ALU.mult, None, [pd, gates], [ai])
                P.dma(acc.t[rs, cs], ai[:], reads=[ai], writes=[acc.sub(key)], semtrk=ai)
                i2 += 1
    P.end_phase()

    P.begin_phase()
    gf = P.sb([128, D], F32, "gf")
    P.dma(gf[:], modv.t[3:4, :].partition_broadcast(128), reads=[modv], writes=[gf], semtrk=gf)
    fg = P.sb([128, D], F32, "fg")
    P.dma(fg[:], fing.t.rearrange("(o n) -> o n", o=1).partition_broadcast(128), reads=[fing], writes=[fg],
          semtrk=fg)
    xa = [P.sb([128, D], F32, f"xa{i}") for i in range(2)]
    ab_ = [P.sb([128, D], F32, f"ab{i}") for i in range(2)]
    junk = P.sb([128, D], BF16, "junk4")
    ss = P.sb([128, 1], F32, "ss4")
    ms = P.sb([128, 1], F32, "ms4")
    rstd = P.sb([128, 1], F32, "rstd4")
    for tt in range(NTT):
        rs = slice(tt * 128, (tt + 1) * 128)
        a = xa[tt % 2]
        b = ab_[tt % 2]
        P.dma(a[:], x1.t[rs, :], reads=[x1.sub(tt)], writes=[a], semtrk=a)
        P.dma(b[:], acc.t[rs, :], reads=[acc.sub((tt, ct)) for ct in range(8)], writes=[b], semtrk=b)
        P.tt(b[:], b[:], gf[:], ALU.mult, [b, gf], [b], eng="gpsimd")
        P.tt(a[:], a[:], b[:], ALU.add, [a, b], [a])
        if out is not None:
            P.dma(out.t[rs, :], a[:], reads=[a], writes=[out], semtrk=a)
        if outn is None:
            continue
        P.act(junk[:], a[:], AF.Square, [a], [junk, ss], accum_out=ss[:])
        P.ts(ms[:], ss[:], 1.0 / D, EPS, ALU.mult, ALU.add, [ss], [ms])
        P.act(ms[:], ms[:], AF.Sqrt, [ms], [ms])
        P.op("vector", lambda e: e.reciprocal(rstd[:], ms[:]), [ms], [rstd])
        P.act(b[:], a[:], AF.Identity, [a, rstd], [b], scale=rstd[:])
        P.tt(b[:], b[:], fg[:], ALU.mult, [b, fg], [b], eng="gpsimd")
        P.dma(outn.t[rs, :], b[:], reads=[b], writes=[outn], semtrk=b)
    P.end_phase()
    if ctx is not None:
        return None
    return P.finish()


def run_B(l, last, x, yT_full, mod, inp, n_exp=NE):
    nc = build_B(n_exp)
    ident = np.eye(128, dtype=np.float32)
    in_maps = []
    for i in range(8):
        b = i // 4
        m = mod[l, b].reshape(6, D)
        modv = np.ascontiguousarray(np.stack([m[2], m[3], m[4], m[5]]))
        in_maps.append({
            "x": np.ascontiguousarray(x[i * TPC:(i + 1) * TPC]),
            "yT": np.ascontiguousarray(yT_full[i]),
            "w_out": inp["w_out"][l], "modv": modv, "g_ffn": inp["norm_ffn_g"][l],
            "final_g": inp["final_g"], "w_router": inp["w_router"], "router_bias": inp["router_bias"],
            "w_gate": inp["w_gate_e"][l], "w_up": inp["w_up_e"][l], "w_down": inp["w_down_e"][l],
            "ident": ident,
        })
    res = run_bass_kernel_spmd(nc, in_maps, core_ids=list(range(8)))
    return np.concatenate([r["outn" if last else "out"] for r in res.results], axis=0)


NFM = 18
WC = NFM * 128 + 3 + 384 + 384 + 6
TMW = 774
CT_TOK = 1152


def emit_hT(P, x_d, ntiles, hT_d, AT, BvT, ident, bufs, rowsrc=None):
    xrs, junks, xss, hbs, pst, sss, mss, rstds = bufs

    def stage1(tt):
        xr = xrs[tt % 2]
        junk, xs, ss, ms, rstd = junks[tt % 2], xss[tt % 2], sss[tt % 2], mss[tt % 2], rstds[tt % 2]
        rs = slice(tt * 128, (tt + 1) * 128)
        if rowsrc is None:
            P.dma(xr[:], x_d.t[rs, :], reads=[x_d], writes=[xr], semtrk=xr)
        else:
            P.dma_group([(xr[p0:p1, :], ap) for (p0, p1, ap) in rowsrc(tt)], reads=[x_d], writes=[xr], semtrk=xr)
        P.act(junk[:], xr[:], AF.Square, [xr], [junk, ss], accum_out=ss[:])
        P.ts(ms[:], ss[:], 1.0 / D, EPS, ALU.mult, ALU.add, [ss], [ms])
        P.act(ms[:], ms[:], AF.Sqrt, [ms], [ms])
        P.op("vector", lambda e, rstd=rstd, ms=ms: e.reciprocal(rstd[:], ms[:]), [ms], [rstd])
        P.act(xs[:], xr[:], AF.Identity, [xr, rstd], [xs], scale=rstd[:])

    def stage2(tt):
        xs = xss[tt % 2]
        rs = slice(tt * 128, (tt + 1) * 128)
        hb = hbs[tt % 2]
        for kq in range(KC // 4):
            ps = pst[kq % 4]
            for j in range(4):
                k = kq * 4 + j
                P.op("tensor", lambda e, ps=ps, j=j, k=k, xs=xs: e.transpose(ps[:, j * 128:(j + 1) * 128],
                                                                            xs[:, k * 128:(k + 1) * 128], ident[:]),
                     [xs, ident], [ps])
            for j in range(4):
                k = kq * 4 + j
                if j % 2 == 0:
                    P.ts(hb[:, k, :], ps[:, j * 128:(j + 1) * 128], AT[:, k:k + 1], BvT[:, k:k + 1], ALU.mult,
                         ALU.add, [ps, AT, BvT], [hb])
                else:
                    P.act(hb[:, k, :], ps[:, j * 128:(j + 1) * 128], AF.Identity, [ps, AT, BvT], [hb],
                          scale=AT[:, k:k + 1], bias=BvT[:, k:k + 1])
        P.dma(hT_d.t[:, :, rs], hb[:], reads=[hb], writes=[hT_d.sub(tt // 4)], semtrk=hb)

    stage1(0)
    for tt in range(ntiles):
        if tt + 1 < ntiles:
            stage1(tt + 1)
        stage2(tt)


def build_A(stop=9, ctx=None):
    IN, OUT, SCR = "ExternalInput", "ExternalOutput", "Internal"
    if ctx is not None:
        P = ctx["P"]
        nc = P.nc
        (x_b, x_c, wc, wa, modm, gmix, cwT, cvec, halo, hv, fb3, dnp, dcw, ident_d, ut_d, cm_d, ybT, ycT, yaT,
         hTd, hTc, projT, ffd, Fd, projM, aT) = (
            ctx[k] for k in ("x_b", "x_c", "wc", "wa", "modm", "gmix", "cwT", "cvec", "halo", "hv", "fb3", "dnp",
                             "dcw", "ident", "ut", "cmask", "ybT", "ycT", "yaT", "hTd", "hTc", "projT", "ffd",
                             "Fd", "projM", "aT"))
    else:
        P = Prog()
        nc = P.nc
        x_b = P.dram("x_b", [SEQ, D], F32, IN)
        x_c = P.dram("x_c", [CT_TOK, D], F32, IN)
        wc = P.dram("wc", [D, WC], F32, IN)
        wa = P.dram("wa", [D, 2048], F32, IN)
        modm = P.dram("modm", [2, D], F32, IN)
        gmix = P.dram("g_mix", [D], F32, IN)
        cwT = P.dram("cwT", [CONV_CH, 31], F32, IN)
        cvec = P.dram("cvec", [3, CONV_CH], F32, IN)
        halo = P.dram("halo", [128, 1], F32, IN)
        hv = P.dram("hv", [4, 128], F32, IN)
        fb3 = P.dram("fb3", [3, 1], F32, IN)
        dnp = P.dram("dnp", [2, 4], F32, IN)
        dcw = P.dram("dcw", [9 * 128, 4], F32, IN)
        ident_d = P.dram("ident", [128, 128], F32, IN)
        ut_d = P.dram("ut", [128, 128], F32, IN)
        cm_d = P.dram("cmask", [128, 4, 512], BF16, IN)
        ybT = P.dram("ybT", [384, SEQ], BF16, OUT)
        ycT = P.dram("ycT", [384, SEQ], BF16, OUT)
        yaT = P.dram("yaT", [CONV_CH, 1024], BF16, OUT)
        hTd = P.dram("hTd", [128, KC, SEQ], BF16, SCR)
        hTc = P.dram("hTc", [128, KC, CT_TOK], BF16, SCR)
        projT = P.dram("projT", [NFM, 128, SEQ], F32, SCR)
        ffd = P.dram("ffd", [3, SEQ], F32, SCR)
        Fd = P.dram("Fd", [3, SEQ], F32, SCR)
        projM = P.dram("projM", [SEQ, TMW], F32, SCR)
        aT = P.dram("aT", [16, 128, CT_TOK], F32, SCR)

    P.begin_phase()
    ident = P.sb([128, 128], F32, "ident")
    P.dma(ident[:], ident_d.t, reads=[ident_d], writes=[ident], semtrk=ident)
    gT = P.sb([128, KC], F32, "gT")
    scT = P.sb([128, KC], F32, "scT")
    BvT = P.sb([128, KC], F32, "BvT")
    AT = P.sb([128, KC], F32, "AT")
    P.dma(gT[:], gmix.t.rearrange("(k p) -> p k", p=128), reads=[gmix], writes=[gT], semtrk=gT,
          allow_slow_non_contiguous=True)
    P.dma(scT[:], modm.t[1].rearrange("(k p) -> p k", p=128), reads=[modm], writes=[scT], semtrk=scT,
          allow_slow_non_contiguous=True)
    P.dma(BvT[:], modm.t[0].rearrange("(k p) -> p k", p=128), reads=[modm], writes=[BvT], semtrk=BvT,
          allow_slow_non_contiguous=True)
    P.stt(AT[:], scT[:], 1.0, gT[:], ALU.add, ALU.mult, [scT, gT], [AT])
    bufs = ([P.sb([128, D], F32, f"xr{i}") for i in range(2)], [P.sb([128, D], BF16, f"junk{i}") for i in range(2)],
            [P.sb([128, D], F32, f"xs{i}") for i in range(2)], [P.sb([128, KC, 128], BF16, f"hb{i}") for i in range(2)],
            [P.ps([128, 512], F32, f"pst{i}") for i in range(4)],
            [P.sb([128, 1], F32, f"ss{i}") for i in range(2)], [P.sb([128, 1], F32, f"ms{i}") for i in range(2)],
            [P.sb([128, 1], F32, f"rstd{i}") for i in range(2)])
    emit_hT(P, x_b, SEQ // 128, hTd, AT, BvT, ident, bufs, rowsrc=(ctx or {}).get("x_b_rows"))
    emit_hT(P, x_c, CT_TOK // 128, hTc, AT, BvT, ident, bufs)
    P.end_phase()

    if stop < 2:
        return P.finish()
    P.begin_phase()
    wbs = [P.sb([128, KC, 512], BF16, f"wb{i}") for i in range(2)]
    hts = [P.sb([128, KC, 512], BF16, f"ht{i}") for i in range(2)]
    evs = [P.sb([128, 512], F32, f"ev{i}") for i in range(4)]
    pss = [P.ps([128, 512], F32, f"ps{i}") for i in range(4)]
    wcv = wc.t.rearrange("(k p) n -> p k n", p=128)
    wav = wa.t.rearrange("(k p) n -> p k n", p=128)
    groups = []
    for g0 in range(0, NFM, 4):
        groups.append(("FM", g0 * 128, min(4, NFM - g0) * 128, g0))
    groups.append(("FF", NFM * 128, 3, 0))
    groups.append(("TM", NFM * 128 + 3, 384, 0))
    groups.append(("TM", NFM * 128 + 3 + 384, 390, 384))
    cnt = {"w": 0, "h": 0, "e": 0}

    def evac(ps_ap, n_part, ncols, dst_ap, dst_trk):
        ev = evs[cnt["e"] % 4]
        if cnt["e"] % 2 == 0:
            P.act(ev[:n_part, :ncols], ps_ap, AF.Copy, [pss[cnt["p"] % 4]], [ev])
        else:
            P.copy(ev[:n_part, :ncols], ps_ap, [pss[cnt["p"] % 4]], [ev], eng="vector")
        P.dma(dst_ap, ev[:n_part, :ncols], reads=[ev], writes=[dst_trk], semtrk=ev)
        cnt["e"] += 1

    cnt["p"] = 0
    for kind, c0, ncols, aux in groups:
        wb = wbs[cnt["w"] % 2]
        cnt["w"] += 1
        P.dma_group([(wb[:, :16, :ncols], wcv[:, :16, c0:c0 + ncols]), (wb[:, 16:, :ncols], wcv[:, 16:, c0:c0 + ncols])],
                    reads=[wc], writes=[wb], semtrk=wb, q="gpsimd")
        for t in range(SEQ // 512):
            ht = hts[cnt["h"] % 2]
            cnt["h"] += 1
            tsl = slice(t * 512, (t + 1) * 512)
            P.dma(ht[:], hTd.t[:, :, tsl], reads=[hTd.sub(t)], writes=[ht], semtrk=ht)
            if kind == "FM":
                for ui in range(ncols // 128):
                    ps = pss[cnt["p"] % 4]
                    for k in range(KC):
                        P.mm(ps[:], wb[:, k, ui * 128:(ui + 1) * 128], ht[:, k, :], k == 0, k == KC - 1, [wb, ht], [ps])
                    evac(ps[:], 128, 512, projT.t[aux + ui, :, tsl], projT.sub((aux + ui, t)))
                    cnt["p"] += 1
            elif kind == "FF":
                ps = pss[cnt["p"] % 4]
                for k in range(KC):
                    P.mm(ps[:3, :], wb[:, k, 0:3], ht[:, k, :], k == 0, k == KC - 1, [wb, ht], [ps])
                evac(ps[:3, :], 3, 512, ffd.t[:, tsl], ffd)
                cnt["p"] += 1
            else:
                for s4 in range(4):
                    ps = pss[cnt["p"] % 4]
                    for k in range(KC):
                        P.mm(ps[:, :ncols], ht[:, k, s4 * 128:(s4 + 1) * 128], wb[:, k, :ncols], k == 0, k == KC - 1,
                             [wb, ht], [ps])
                    r0 = t * 512 + s4 * 128
                    evac(ps[:, :ncols], 128, ncols, projM.t[r0:r0 + 128, aux:aux + ncols], projM.sub((aux, t)))
                    cnt["p"] += 1
    for g in range(4):
        wb = wbs[cnt["w"] % 2]
        cnt["w"] += 1
        c0 = g * 512
        P.dma_group([(wb[:, :16, :], wav[:, :16, c0:c0 + 512]), (wb[:, 16:, :], wav[:, 16:, c0:c0 + 512])],
                    reads=[wa], writes=[wb], semtrk=wb, q="gpsimd")
        for t in range(3):
            ht = hts[cnt["h"] % 2]
            cnt["h"] += 1
            tsl = slice(t * 384, (t + 1) * 384)
            P.dma(ht[:, :, :384], hTc.t[:, :, tsl], reads=[hTc.sub(i) for i in range(3)], writes=[ht], semtrk=ht)
            for ui in range(4):
                ps = pss[cnt["p"] % 4]
                for k in range(KC):
                    P.mm(ps[:, :384], wb[:, k, ui * 128:(ui + 1) * 128], ht[:, k, :384], k == 0, k == KC - 1, [wb, ht], [ps])
                evac(ps[:, :384], 128, 384, aT.t[g * 4 + ui, :, tsl], aT.sub(g * 4 + ui))
                cnt["p"] += 1
    P.end_phase()

    if stop < 3:
        return P.finish()
    P.begin_phase()
    ones32 = P.sb([128, 128], F32, "ones32")
    P.memset(ones32[:], 1.0, [ones32])
    cw = P.sb([128, 8, 31], F32, "cw")
    P.dma(cw[:], cwT.t.rearrange("(u p) j -> p u j", p=128), reads=[cwT], writes=[cw], semtrk=cw)
    cv3 = P.sb([128, 3, 8], F32, "cv3")
    P.dma_group([(cv3[:, i, :], cvec.t[i].rearrange("(u p) -> p u", p=128)) for i in range(3)],
                reads=[cvec], writes=[cv3], semtrk=cv3, allow_slow_non_contiguous=True)
    hal = P.sb([128, 1], F32, "hal")
    P.dma(hal[:], halo.t, reads=[halo], writes=[hal], semtrk=hal)
    cvs = [P.sb([128, 1024], F32, f"cv{u}") for u in range(8)]
    avs = [P.sb([128, CT_TOK], F32, f"av{i}") for i in range(2)]
    ags = [P.sb([128, CT_TOK], F32, f"ag{i}") for i in range(2)]
    acc2 = P.sb([128, 1024], F32, "acc2")
    sq = P.sb([128, 512], F32, "sq")
    stat = {n: P.sb([128, 1024], F32, n) for n in ("mean", "rstd", "t1")}
    yo = [P.sb([128, 1024], BF16, f"yo{i}") for i in range(2)]
    psS = [P.ps([128, 512], F32, f"psS{i}") for i in range(2)]
    psQ = [P.ps([128, 512], F32, f"psQ{i}") for i in range(2)]
    for u in range(8):
        av = avs[u % 2]
        ag = ags[u % 2]
        P.dma(av[:], aT.t[u], reads=[aT.sub(u)], writes=[av], semtrk=av)
        P.dma(ag[:], aT.t[8 + u], reads=[aT.sub(8 + u)], writes=[ag], semtrk=ag)
        P.act(ag[:], ag[:], AF.Sigmoid, [ag], [ag])
        P.tt(av[:], av[:], ag[:], ALU.mult, [av, ag], [av])
        P.ts(av[:, :128], av[:, :128], hal[:], None, ALU.mult, None, [av, hal], [av])
        cvu = cvs[u]
        P.ts(cvu[:], av[:, 98:98 + 1024], cw[:, u, 0:1], cv3[:, 0, u:u + 1], ALU.mult, ALU.add, [av, cw, cv3], [cvu])
        for j in range(1, 31):
            P.stt(cvu[:], av[:, 98 + j:98 + j + 1024], cw[:, u, j:j + 1], cvu[:], ALU.mult, ALU.add,
                  [av, cw, cvu], [cvu])
    for th in range(2):
        tsl = slice(th * 512, (th + 1) * 512)
        for u in range(8):
            P.mm(psS[th][:], ones32[:], cvs[u][:, tsl], u == 0, u == 7, [ones32, cvs[u]], [psS[th]])
        for u in range(8):
            P.act(sq[:], cvs[u][:, tsl], AF.Square, [cvs[u]], [sq])
            P.mm(psQ[th][:], ones32[:], sq[:], u == 0, u == 7, [ones32, sq], [psQ[th]])
        mean, rstd, t1 = stat["mean"], stat["rstd"], stat["t1"]
        P.ts(mean[:, tsl], psS[th][:], 1.0 / CONV_CH, None, ALU.mult, None, [psS[th]], [mean])
        P.tt(t1[:, tsl], mean[:, tsl], mean[:, tsl], ALU.mult, [mean], [t1])
        P.stt(t1[:, tsl], psQ[th][:], 1.0 / CONV_CH, t1[:, tsl], ALU.mult, ALU.subtract, [psQ[th], t1], [t1])
        P.ts(t1[:, tsl], t1[:, tsl], EPS, None, ALU.add, None, [t1], [t1])
        P.act(t1[:, tsl], t1[:, tsl], AF.Sqrt, [t1], [t1])
        P.op("vector", lambda e, tsl=tsl: e.reciprocal(rstd[:, tsl], t1[:, tsl]), [t1], [rstd])
    for u in range(8):
        t1 = stat["t1"]
        y = yo[u % 2]
        P.tt(t1[:], cvs[u][:], stat["mean"][:], ALU.subtract, [cvs[u], stat["mean"]], [t1])
        P.tt(t1[:], t1[:], stat["rstd"][:], ALU.mult, [t1, stat["rstd"]], [t1], eng="gpsimd")
        P.act(y[:], t1[:], AF.Silu, [t1, cv3], [y], scale=cv3[:, 1, u:u + 1], bias=cv3[:, 2, u:u + 1])
        P.dma(yaT.t[u * 128:(u + 1) * 128, :], y[:], reads=[y], writes=[yaT], semtrk=y)
    P.end_phase()

    import os
    if stop >= 4 and not os.environ.get("A_SKIPFOX"):
        build_fox(P, projT, projM, ffd, Fd, hv, fb3, cm_d, ybT)
    if stop >= 5:
        build_dn(P, projT, projM, hv, dnp, dcw, ident_d, ut_d, ycT)
    if ctx is not None:
        return None
    return P.finish()


def build_fox(P, projT, projM, ffd, Fd, hv, fb3, cm_d, ybT):
    P.begin_phase()
    ones32 = P.sb([128, 128], F32, "ones32")
    P.memset(ones32[:], 1.0, [ones32])
    onesb = P.sb([128, 128], BF16, "onesb")
    P.memset(onesb[:], 1.0, [onesb])
    cmask = P.sb([128, 4, 512], BF16, "cmask")
    P.dma(cmask[:], cm_d.t, reads=[cm_d], writes=[cmask], semtrk=cmask)
    gains = P.sb([128, 4], F32, "gains")
    P.dma(gains[:], hv.t.rearrange("r p -> p r"), reads=[hv], writes=[gains], semtrk=gains,
          allow_slow_non_contiguous=True)
    fa = P.sb([3, SEQ], F32, "fa")
    fbuf = P.sb([3, SEQ], F32, "fbuf")
    nb = P.sb([3, 1], F32, "nb")
    P.dma(fa[:], ffd.t, reads=[ffd], writes=[fa], semtrk=fa)
    P.dma(nb[:], fb3.t, reads=[fb3], writes=[nb], semtrk=nb)
    P.ts(nb[:], nb[:], -1.0, None, ALU.mult, None, [nb], [nb])
    P.act(fa[:], fa[:], AF.Exp, [fa, nb], [fa], bias=nb[:], scale=-1.0)
    P.act(fa[:], fa[:], AF.Ln, [fa], [fa], bias=1.0)
    a, b = fa, fbuf
    s = 1
    while s < SEQ:
        P.tt(b[:, s:], a[:, s:], a[:, :SEQ - s], ALU.add, [a], [b])
        P.copy(b[:, :s], a[:, :s], [a], [b], eng="gpsimd")
        a, b = b, a
        s *= 2
    P.ts(b[:], a[:], -1.0, None, ALU.mult, None, [a], [b])
    P.dma(Fd.t, b[:], reads=[b], writes=[Fd], semtrk=b)
    FT = P.sb([128, 3, 32], F32, "FT")
    Fref = P.sb([128, 3, 32], F32, "Fref")
    P.dma_group([(FT[:, h, :], Fd.t[h].rearrange("(kt p) -> p kt", p=128)) for h in range(3)],
                reads=[Fd], writes=[FT], semtrk=FT, allow_slow_non_contiguous=True)
    P.dma_group([(Fref[:, h, :], Fd.t[h:h + 1, :].rearrange("o (g s) -> o g s", s=128)[:, :, 127].partition_broadcast(128))
                 for h in range(3)], reads=[Fd], writes=[Fref], semtrk=Fref, allow_slow_non_contiguous=True)
    biasT = P.sb([128, 3, 32, 32], F32, "biasT")
    for h in range(3):
        for g in range(32):
            P.ts(biasT[:, h, g, :], FT[:, h, :], -1.0, Fref[:, h, g:g + 1], ALU.mult, ALU.add, [FT, Fref], [biasT])

    qn = P.sb([128, SEQ], BF16, "qn")
    kn = P.sb([128, SEQ], BF16, "kn")
    v32 = P.sb([128, 32, 128], F32, "v32")
    vb = P.sb([128, 32, 128], BF16, "vb")
    xt = [P.sb([128, 512], F32, f"xt{i}") for i in range(2)]
    sqt = [P.sb([128, 512], F32, f"sqt{i}") for i in range(2)]
    rt = [P.sb([128, 512], F32, f"rt{i}") for i in range(2)]
    pT = [P.sb([128, 512], BF16, f"pT{i}") for i in range(3)]
    ot = [P.sb([128, 512], F32, f"ot{i}") for i in range(2)]
    fgt = [P.sb([128, 512], F32, f"fgt{i}") for i in range(2)]
    yt = [P.sb([128, 512], BF16, f"yt{i}") for i in range(2)]
    ps_s = [P.ps([128, 512], F32, f"ps_s{i}") for i in range(3)]
    po = [P.ps([128, 512], F32, f"po{i}") for i in range(2)]
    pden = [P.ps([128, 512], F32, f"pden{i}") for i in range(2)]
    ps_n = P.ps([128, 512], F32, "ps_n")
    ci = 0
    gi = 0
    scale = float(HD) ** -0.5
    for h in range(3):
        for comp, dst, gcol in ((h, qn, 0), (3 + h, kn, 1)):
            for t in range(8):
                tsl = slice(t * 512, (t + 1) * 512)
                x_ = xt[ci % 2]
                sq_ = sqt[ci % 2]
                r_ = rt[ci % 2]
                ci += 1
                P.dma(x_[:], projT.t[comp, :, tsl], reads=[projT.sub((comp, t))], writes=[x_], semtrk=x_)
                P.act(sq_[:], x_[:], AF.Square, [x_], [sq_])
                P.mm(ps_n[:], ones32[:], sq_[:], True, True, [ones32, sq_], [ps_n])
                P.ts(r_[:], ps_n[:], 1.0 / HD, EPS, ALU.mult, ALU.add, [ps_n], [r_])
                P.act(r_[:], r_[:], AF.Sqrt, [r_], [r_])
                P.op("vector", lambda e, r_=r_: e.reciprocal(r_[:], r_[:]), [r_], [r_])
                P.tt(x_[:], x_[:], r_[:], ALU.mult, [x_, r_], [x_], eng="gpsimd")
                P.ts(dst[:, tsl], x_[:], gains[:, gcol:gcol + 1], None, ALU.mult, None, [x_, gains], [dst])
        P.dma(v32[:], projM.t[:, h * 128:(h + 1) * 128].rearrange("(kt p) c -> p kt c", p=128),
              reads=[projM.sub((0, t)) for t in range(8)], writes=[v32], semtrk=v32)
        P.copy(vb[:], v32[:], [v32], [vb], eng="gpsimd")
        for g in range(8):
            gsl = slice(g * 512, (g + 1) * 512)
            po_, pd_ = po[gi % 2], pden[gi % 2]
            nkt = 4 * g + 4
            for kt in range(nkt):
                pss_ = ps_s[kt % 3]
                p_ = pT[kt % 3]
                P.mm(pss_[:], kn[:, kt * 128:(kt + 1) * 128], qn[:, gsl], True, True, [kn, qn], [pss_])
                jd = kt - 4 * g
                for qq in range(4):
                    qs = slice(qq * 128, (qq + 1) * 128)
                    if qq < jd:
                        P.memset(p_[:, qs], 0.0, [p_], eng="gpsimd")
                        continue
                    P.act(p_[:, qs], pss_[:, qs], AF.Exp, [pss_, biasT], [p_], bias=biasT[:, h, 4 * g + qq, kt:kt + 1],
                          scale=scale)
                    if qq == jd:
                        P.tt(p_[:, qs], p_[:, qs], cmask[:, 0, 0:128], ALU.mult, [p_, cmask], [p_], eng="gpsimd")
                P.mm(po_[:], vb[:, kt, :], p_[:], kt == 0, kt == nkt - 1, [vb, p_], [po_])
                P.mm(pd_[:], onesb[:], p_[:], kt == 0, kt == nkt - 1, [onesb, p_], [pd_])
            o_ = ot[gi % 2]
            r_ = rt[gi % 2]
            sq_ = sqt[gi % 2]
            fg_ = fgt[gi % 2]
            y_ = yt[gi % 2]
            P.dma(fg_[:], projT.t[6 + h, :, gsl], reads=[projT.sub((6 + h, g))], writes=[fg_], semtrk=fg_)
            P.op("vector", lambda e, r_=r_, pd_=pd_: e.reciprocal(r_[:], pd_[:]), [pd_], [r_])
            P.tt(o_[:], po_[:], r_[:], ALU.mult, [po_, r_], [o_])
            P.act(sq_[:], o_[:], AF.Square, [o_], [sq_])
            P.mm(ps_n[:], ones32[:], sq_[:], True, True, [ones32, sq_], [ps_n])
            P.ts(r_[:], ps_n[:], 1.0 / HD, EPS, ALU.mult, ALU.add, [ps_n], [r_])
            P.act(r_[:], r_[:], AF.Sqrt, [r_], [r_])
            P.op("vector", lambda e, r_=r_: e.reciprocal(r_[:], r_[:]), [r_], [r_])
            P.act(fg_[:], fg_[:], AF.Sigmoid, [fg_], [fg_])
            P.tt(o_[:], o_[:], r_[:], ALU.mult, [o_, r_], [o_])
            P.tt(o_[:], o_[:], fg_[:], ALU.mult, [o_, fg_], [o_], eng="gpsimd")
            P.ts(y_[:], o_[:], gains[:, 2:3], None, ALU.mult, None, [o_, gains], [y_])
            P.dma(ybT.t[h * 128:(h + 1) * 128, gsl], y_[:], reads=[y_], writes=[ybT], semtrk=y_)
            gi += 1
    P.end_phase()


def build_dn(P, projT, projM, hv, dnp, dcw, ident_d, ut_d, ycT):
    import os
    DS = float(os.environ.get("DN_STOP", "99"))
    P.begin_phase()
    NCH = SEQ // 128
    ident = P.sb([128, 128], F32, "ident")
    ut = P.sb([128, 128], F32, "ut")
    P.dma(ident[:], ident_d.t, reads=[ident_d], writes=[ident], semtrk=ident)
    P.dma(ut[:], ut_d.t, reads=[ut_d], writes=[ut], semtrk=ut)
    ones32 = P.sb([128, 128], F32, "ones32")
    P.memset(ones32[:], 1.0, [ones32])
    gon = P.sb([128, 128], F32, "gon")
    P.dma(gon[:], hv.t[3:4, :].partition_broadcast(128), reads=[hv], writes=[gon], semtrk=gon)
    dcwt = P.sb([128, 9, 4], F32, "dcwt")
    P.dma(dcwt[:], dcw.t.rearrange("(u p) j -> p u j", p=128), reads=[dcw], writes=[dcwt], semtrk=dcwt)
    pbk = [P.ps([128, 512], F32, f"pb{i}") for i in range(4)]
    pmisc = P.ps([128, 512], F32, "pmisc")
    pscan = [P.ps([128, 512], F32, f"pscan{i}") for i in range(2)]

    def slot(pb, s):
        return pb[:, s * 128:(s + 1) * 128]

    Lm = P.sb([128, 128], F32, "Lm")
    SLm = P.sb([128, 128], F32, "SLm")
    P.op("tensor", lambda e: e.transpose(slot(pmisc, 0), ut[:], ident[:]), [ut, ident], [pmisc])
    P.copy(Lm[:], slot(pmisc, 0), [pmisc], [Lm])
    P.tt(SLm[:], Lm[:], ident[:], ALU.subtract, [Lm, ident], [SLm])
    gb6 = P.sb([128, NCH, 6], F32, "gb6")
    P.dma(gb6[:], projM.t[:, 768:774].rearrange("(c p) n -> p c n", p=128),
          reads=[projM.sub((384, t)) for t in range(8)], writes=[gb6], semtrk=gb6)
    pr = P.sb([128, 2, 4], F32, "pr")
    P.dma_group([(pr[:, i, :], dnp.t[i:i + 1, :].partition_broadcast(128)) for i in range(2)],
                reads=[dnp], writes=[pr], semtrk=pr)
    beta = P.sb([128, NCH, 4], F32, "beta")
    g4 = P.sb([128, NCH, 4], F32, "g4")
    P.memset(g4[:], 0.0, [g4])
    P.memset(beta[:], 0.0, [beta])
    P.act(beta[:, :, 0:3], gb6[:, :, 0:3], AF.Sigmoid, [gb6], [beta])
    zt = P.sb([128, NCH, 3], F32, "zt")
    P.tt(zt[:], gb6[:, :, 3:6], pr[:, 1, 0:3].unsqueeze(1).to_broadcast([128, NCH, 3]), ALU.add, [gb6, pr], [zt])
    P.act(zt[:], zt[:], AF.Exp, [zt], [zt])
    P.act(zt[:], zt[:], AF.Ln, [zt], [zt], bias=1.0)
    ea = P.sb([128, 4], F32, "ea")
    P.act(ea[:], pr[:, 0, :], AF.Exp, [pr], [ea])
    P.ts(ea[:], ea[:], -1.0, None, ALU.mult, None, [ea], [ea])
    P.tt(g4[:, :, 0:3], zt[:], ea[:, 0:3].unsqueeze(1).to_broadcast([128, NCH, 3]), ALU.mult, [zt, ea], [g4])

    if DS <= 1:
        P.end_phase()
        return
    xin = P.sb([128, SEQ], F32, "xin")
    ycv = P.sb([128, SEQ], F32, "ycv")
    qTb = P.sb([128, SEQ], BF16, "qTb")
    kTb = P.sb([128, SEQ], BF16, "kTb")
    kT32 = P.sb([128, SEQ], F32, "kT32")
    vT32 = P.sb([128, SEQ], F32, "vT32")
    sq_ = P.sb([128, 512], F32, "sq")
    r_ = P.sb([128, 512], F32, "r")
    u_all = P.sb([128, NCH, 128], F32, "u_all")
    wT_all = P.sb([128, NCH, 128], BF16, "wT_all")
    qkT_all = P.sb([128, NCH, 128], BF16, "qkT_all")
    kdec_all = P.sb([128, NCH, 128], BF16, "kdec_all")
    egc_all = P.sb([128, NCH], F32, "egc_all")
    egl_all = P.sb([128, NCH], F32, "egl_all")
    NBAT = 4
    W = [{n: P.sb([128, 128], F32, f"{n}{i}") for n in
          ("gbm", "gcb", "E", "A", "qk", "B", "M0", "M1", "A2", "B2", "A3", "B3", "bkg", "bv")} for i in range(NBAT)]
    Wc = [{n: P.sb([128, 1], F32, f"{n}{i}") for n in ("gcc", "edec", "bgc", "tmp")} for i in range(NBAT)]
    S32 = P.sb([128, 128], F32, "S32")
    Sb = P.sb([128, 128], BF16, "Sb")
    vnew = [P.sb([128, 128], BF16, f"vnew{i}") for i in range(2)]
    ot = [P.sb([128, 128], F32, f"ot{i}") for i in range(2)]
    t2 = [P.sb([128, 128], F32, f"t2{i}") for i in range(2)]
    dzt = [P.sb([128, 128], F32, f"dzt{i}") for i in range(2)]
    junk = P.sb([128, 128], F32, "junk")
    sc1 = [{n: P.sb([128, 1], F32, f"{n}{i}") for n in ("ss", "ms", "rstd")} for i in range(2)]
    ybuf = [P.sb([128, 512], BF16, f"ybuf{i}") for i in range(2)]

    for h in range(3):
        for comp in range(3):
            u = comp * 3 + h
            P.dma(xin[:], projT.t[9 + u], reads=[projT.sub((9 + u, t)) for t in range(8)], writes=[xin], semtrk=xin)
            P.ts(ycv[:], xin[:], dcwt[:, u, 3:4], None, ALU.mult, None, [xin, dcwt], [ycv])
            for j in (1, 2, 3):
                P.stt(ycv[:, j:], xin[:, :SEQ - j], dcwt[:, u, 3 - j:4 - j], ycv[:, j:], ALU.mult, ALU.add,
                      [xin, dcwt, ycv], [ycv])
            if comp == 2:
                P.act(vT32[:], ycv[:], AF.Silu, [ycv], [vT32])
                continue
            P.act(ycv[:], ycv[:], AF.Silu, [ycv], [ycv])
            for t in range(8):
                tsl = slice(t * 512, (t + 1) * 512)
                P.act(sq_[:], ycv[:, tsl], AF.Square, [ycv], [sq_])
                P.mm(pmisc[:], ones32[:], sq_[:], True, True, [ones32, sq_], [pmisc])
                P.ts(r_[:], pmisc[:], EPS, None, ALU.add, None, [pmisc], [r_])
                P.act(r_[:], r_[:], AF.Sqrt, [r_], [r_])
                P.op("vector", lambda e: e.reciprocal(r_[:], r_[:]), [r_], [r_])
                if comp == 0:
                    P.stt(qTb[:, tsl], ycv[:, tsl], float(HD) ** -0.5, r_[:], ALU.mult, ALU.mult, [ycv, r_], [qTb])
                else:
                    P.tt(kT32[:, tsl], ycv[:, tsl], r_[:], ALU.mult, [ycv, r_], [kT32])
                    P.copy(kTb[:, tsl], kT32[:, tsl], [kT32], [kTb], eng="gpsimd")
        if DS <= 2:
            P.end_phase()
            return
        for c0 in range(0, NCH, NBAT):
            chains = list(range(c0, c0 + NBAT))

            def each(f):
                for i, c in enumerate(chains):
                    f(i, c, W[i], Wc[i], pbk[i], slice(c * 128, (c + 1) * 128))

            def s1(i, c, w, wc, pb, csl):
                P.ts(w["gbm"][:], ones32[:], g4[:, c, h:h + 1], None, ALU.mult, None, [ones32, g4], [w["gbm"]])
                P.mm(slot(pb, 0), w["gbm"][:], ut[:], True, True, [w["gbm"], ut], [pb])
                P.mm(pb[:, 128:132], ut[:], g4[:, c, :], True, True, [ut, g4], [pb])
                P.mm(slot(pb, 2), kTb[:, csl], kTb[:, csl], True, True, [kTb], [pb])
                P.mm(slot(pb, 3), qTb[:, csl], kTb[:, csl], True, True, [qTb, kTb], [pb])
            each(s1)

            def s2(i, c, w, wc, pb, csl):
                P.copy(w["gcb"][:], slot(pb, 0), [pb], [w["gcb"]], eng="scalar")
                P.copy(wc["gcc"][:], pb[:, 128 + h:129 + h], [pb], [wc["gcc"]])
                P.ts(w["E"][:], w["gcb"][:], -1.0, wc["gcc"][:], ALU.mult, ALU.add, [w["gcb"], wc["gcc"]], [w["E"]])
                P.ts(w["E"][:], w["E"][:], 0.0, None, ALU.min, None, [w["E"]], [w["E"]])
                P.act(w["E"][:], w["E"][:], AF.Exp, [w["E"]], [w["E"]])
                P.tt(wc["tmp"][:], w["gcb"][:, 127:128], wc["gcc"][:], ALU.subtract, [w["gcb"], wc["gcc"]], [wc["tmp"]])
                P.act(wc["edec"][:], wc["tmp"][:], AF.Exp, [wc["tmp"]], [wc["edec"]])
                P.act(egc_all[:, c:c + 1], wc["gcc"][:], AF.Exp, [wc["gcc"]], [egc_all])
                P.act(egl_all[:, c:c + 1], w["gcb"][:, 127:128], AF.Exp, [w["gcb"]], [egl_all])
                P.tt(wc["bgc"][:], beta[:, c, h:h + 1], egc_all[:, c:c + 1], ALU.mult, [beta, egc_all], [wc["bgc"]])
                P.tt(w["A"][:], slot(pb, 2), w["E"][:], ALU.mult, [pb, w["E"]], [w["A"]])
                P.stt(w["A"][:], w["A"][:], beta[:, c, h:h + 1], SLm[:], ALU.mult, ALU.mult, [w["A"], beta, SLm], [w["A"]])
                P.tt(w["qk"][:], slot(pb, 3), w["E"][:], ALU.mult, [pb, w["E"]], [w["qk"]])
                P.tt(w["qk"][:], w["qk"][:], Lm[:], ALU.mult, [w["qk"], Lm], [w["qk"]], eng="gpsimd")
            each(s2)
            if DS <= 3:
                P.end_phase()
                return

            DBG = os.environ.get("DN_DBG", "")

            def s3(i, c, w, wc, pb, csl):
                if "1" in DBG and i > 0:
                    return
                if "A" not in DBG:
                    P.op("tensor", lambda e: e.transpose(slot(pb, 0), w["A"][:], ident[:]), [w["A"], ident], [pb])
                if "Q" not in DBG:
                    P.op("tensor", lambda e: e.transpose(slot(pb, 1), w["qk"][:], ident[:]), [w["qk"], ident], [pb])
            each(s3)
            if DS <= 3.1:
                P.end_phase()
                return

            def s4(i, c, w, wc, pb, csl):
                if "a" not in DBG:
                    P.copy(w["B"][:], slot(pb, 0), [pb], [w["B"]], eng="scalar")
                if "v" not in DBG:
                    P.copy(qkT_all[:, c, :], slot(pb, 1), [pb], [qkT_all])
                if "p" not in DBG:
                    P.tt(w["M0"][:], ident[:], w["B"][:], ALU.subtract, [ident, w["B"]], [w["M0"]], eng="gpsimd")
            each(s4)
            if DS <= 3.3:
                P.end_phase()
                return
            names = [("A", "B", "A2", "B2"), ("A2", "B2", "A3", "B3"), ("A3", "B3", "A2", "B2")]
            for lvl in range(6):
                na, nb_, na2, nb2 = names[0] if lvl == 0 else names[1 + (lvl - 1) % 2]
                mi, mo = ("M0", "M1") if lvl % 2 == 0 else ("M1", "M0")

                def q1(i, c, w, wc, pb, csl):
                    P.mm(slot(pb, 2), w[nb_][:], w[na][:], True, True, [w[nb_], w[na]], [pb])
                    if lvl < 5:
                        P.mm(slot(pb, 3), w[na][:], w[nb_][:], True, True, [w[nb_], w[na]], [pb])
                each(q1)

                def q2(i, c, w, wc, pb, csl):
                    P.copy(w[na2][:], slot(pb, 2), [pb], [w[na2]], eng="scalar")
                    if lvl < 5:
                        P.copy(w[nb2][:], slot(pb, 3), [pb], [w[nb2]])
                each(q2)

                def q3(i, c, w, wc, pb, csl):
                    P.mm(slot(pb, 0), w[na2][:], w[mi][:], True, True, [w[na2], w[mi]], [pb])
                each(q3)

                def q4(i, c, w, wc, pb, csl):
                    P.tt(w[mo][:], w[mi][:], slot(pb, 0), ALU.add, [w[mi], pb], [w[mo]])
                each(q4)
            if DS <= 3.6:
                P.end_phase()
                return
            mf = "M0"

            def s5(i, c, w, wc, pb, csl):
                P.op("tensor", lambda e: e.transpose(slot(pb, 1), kT32[:, csl], ident[:]), [kT32, ident], [pb])
                P.op("tensor", lambda e: e.transpose(slot(pb, 2), vT32[:, csl], ident[:]), [vT32, ident], [pb])
            each(s5)

            def s6(i, c, w, wc, pb, csl):
                P.ts(w["bkg"][:], slot(pb, 1), wc["bgc"][:], None, ALU.mult, None, [pb, wc["bgc"]], [w["bkg"]])
                P.ts(kdec_all[:, c, :], slot(pb, 1), wc["edec"][:], None, ALU.mult, None, [pb, wc["edec"]], [kdec_all],
                     eng="gpsimd" if False else "vector")
                P.ts(w["bv"][:], slot(pb, 2), beta[:, c, h:h + 1], None, ALU.mult, None, [pb, beta], [w["bv"]])
            each(s6)

            def s7(i, c, w, wc, pb, csl):
                P.mm(slot(pb, 3), w[mf][:], w["bv"][:], True, True, [w[mf], w["bv"]], [pb])
                P.mm(slot(pb, 0), w["bkg"][:], w[mf][:], True, True, [w[mf], w["bkg"]], [pb])
            each(s7)

            def s8(i, c, w, wc, pb, csl):
                P.copy(u_all[:, c, :], slot(pb, 3), [pb], [u_all], eng="scalar")
                P.copy(wT_all[:, c, :], slot(pb, 0), [pb], [wT_all])
            each(s8)
            if DS <= 4:
                P.end_phase()
                return
        if DS <= 5:
            P.end_phase()
            return
        P.memset(S32[:], 0.0, [S32])
        P.memset(Sb[:], 0.0, [Sb], eng="gpsimd")
        for c in range(NCH):
            csl = slice(c * 128, (c + 1) * 128)
            pb = pscan[c % 2]
            vn = vnew[c % 2]
            o_ = ot[c % 2]
            t_ = t2[c % 2]
            dz_ = dzt[c % 2]
            s_ = sc1[c % 2]
            yb = ybuf[(c // 4) % 2]
            P.dma(dz_[:], projM.t[csl, 384 + h * 128:384 + (h + 1) * 128], reads=[projM.sub((384, c // 4))],
                  writes=[dz_], semtrk=dz_)
            P.mm(slot(pb, 0), wT_all[:, c, :], Sb[:], True, True, [wT_all, Sb], [pb])
            P.mm(slot(pb, 1), qTb[:, csl], Sb[:], True, True, [qTb, Sb], [pb])
            P.tt(vn[:], u_all[:, c, :], slot(pb, 0), ALU.subtract, [u_all, pb], [vn])
            P.mm(slot(pb, 2), qkT_all[:, c, :], vn[:], True, True, [qkT_all, vn], [pb])
            P.mm(slot(pb, 3), kdec_all[:, c, :], vn[:], True, True, [kdec_all, vn], [pb])
            P.stt(S32[:], S32[:], egl_all[:, c:c + 1], slot(pb, 3), ALU.mult, ALU.add, [S32, egl_all, pb], [S32])
            P.copy(Sb[:], S32[:], [S32], [Sb], eng="scalar")
            P.act(t_[:], slot(pb, 1), AF.Identity, [pb, egc_all], [t_], scale=egc_all[:, c:c + 1])
            P.tt(o_[:], t_[:], slot(pb, 2), ALU.add, [t_, pb], [o_], eng="vector")
            P.act(junk[:], o_[:], AF.Square, [o_], [junk, s_["ss"]], accum_out=s_["ss"][:])
            P.ts(s_["ms"][:], s_["ss"][:], 1.0 / HD, EPS, ALU.mult, ALU.add, [s_["ss"]], [s_["ms"]], eng="gpsimd")
            P.act(s_["ms"][:], s_["ms"][:], AF.Sqrt, [s_["ms"]], [s_["ms"]])
            P.op("vector", lambda e, s_=s_: e.reciprocal(s_["rstd"][:], s_["ms"][:]), [s_["ms"]], [s_["rstd"]])
            P.act(dz_[:], dz_[:], AF.Silu, [dz_], [dz_])
            P.stt(t_[:], o_[:], s_["rstd"][:], gon[:], ALU.mult, ALU.mult, [o_, s_["rstd"], gon], [t_])
            P.tt(t_[:], t_[:], dz_[:], ALU.mult, [t_, dz_], [t_], eng="gpsimd")
            P.op("tensor", lambda e, t_=t_, c=c: e.transpose(slot(pmisc, c % 4), t_[:], ident[:]), [t_, ident], [pmisc])
            P.copy(yb[:, (c % 4) * 128:(c % 4 + 1) * 128], slot(pmisc, c % 4), [pmisc], [yb], eng="gpsimd" if False else "vector")
            if c % 4 == 3:
                P.dma(ycT.t[h * 128:(h + 1) * 128, (c // 4) * 512:(c // 4 + 1) * 512], yb[:], reads=[yb], writes=[ycT],
                      semtrk=yb)
    P.end_phase()


def host_consts():
    ident = np.eye(128, dtype=np.float32)
    ut = np.triu(np.ones((128, 128), np.float32))
    k = np.arange(128)[:, None, None]
    j = np.arange(4)[None, :, None]
    qq = np.arange(512)[None, None, :]
    cmask = ((j * 128 + k) <= qq).astype(ml_dtypes.bfloat16)
    return ident, ut, cmask


def run_A(l, x3, mod, inp):
    import os
    nc = build_A(int(os.environ.get("A_STOP", "9")))
    ident, ut, cmask = host_consts()
    w_in = inp["w_in"][l]
    in_maps = []
    for i in range(8):
        b, q = i // 4, i % 4
        hs = [3 * q + hh for hh in range(3)]
        cols = []
        for base in (O_FQ, O_FK, O_FG, O_DQ, O_DK, O_DV):
            for hh in hs:
                cols.append(np.arange(base + hh * 128, base + (hh + 1) * 128))
        cols.append(np.array([O_FF + hh for hh in hs]))
        for base in (O_FV, O_DZ):
            for hh in hs:
                cols.append(np.arange(base + hh * 128, base + (hh + 1) * 128))
        cols.append(np.array([O_DB + hh for hh in hs]))
        cols.append(np.array([O_DA + hh for hh in hs]))
        cols = np.concatenate(cols)
        assert cols.size == WC
        xc = np.zeros((CT_TOK, D), np.float32)
        if q == 0:
            xc[128:] = x3[b, :1024]
        else:
            xc[:] = x3[b, q * 1024 - 128:(q + 1) * 1024]
        m = mod[l, b].reshape(6, D)
        dcw_rows = []
        for comp in range(3):
            for hh in hs:
                dcw_rows.append(inp["dn_conv_w"][l][:, comp * DN_W + hh * 128: comp * DN_W + (hh + 1) * 128].T)
        dnp = np.zeros((2, 4), np.float32)
        dnp[0, :3] = inp["dn_a_log"][l][hs]
        dnp[1, :3] = inp["dn_dt_bias"][l][hs]
        in_maps.append({
            "x_b": np.ascontiguousarray(x3[b]), "x_c": xc,
            "wc": np.ascontiguousarray(w_in[:, cols]), "wa": np.ascontiguousarray(w_in[:, :2048]),
            "modm": np.ascontiguousarray(np.stack([m[0], m[1]])), "g_mix": inp["norm_mix_g"][l],
            "cwT": np.ascontiguousarray(inp["conv_w"][l].T),
            "cvec": np.ascontiguousarray(np.stack([inp["conv_b"][l], inp["conv_ln_g"][l], inp["conv_ln_b"][l]])),
            "halo": np.full((128, 1), 0.0 if q == 0 else 1.0, np.float32),
            "hv": np.ascontiguousarray(np.stack([inp["fox_qn_g"][l], inp["fox_kn_g"][l], inp["fox_on_g"][l],
                                                 inp["dn_on_g"][l]])),
            "fb3": np.ascontiguousarray(inp["fox_f_bias"][l][hs].reshape(3, 1)),
            "dnp": dnp, "dcw": np.ascontiguousarray(np.concatenate(dcw_rows, axis=0)),
            "ident": ident, "ut": ut, "cmask": cmask,
        })
    res = run_bass_kernel_spmd(nc, in_maps, core_ids=list(range(8)))
    outs = res.results
    yT_full = np.zeros((8, D, TPC), ml_dtypes.bfloat16)
    for i in range(8):
        b, q = i // 4, i % 4
        yT_full[i, :CONV_CH] = outs[i]["yaT"]
        for qq in range(4):
            src = outs[b * 4 + qq]
            yT_full[i, CONV_CH + qq * 384: CONV_CH + (qq + 1) * 384] = src["ybT"][:, q * TPC:(q + 1) * TPC]
            yT_full[i, CONV_CH + FOX_W + qq * 384: CONV_CH + FOX_W + (qq + 1) * 384] = src["ycT"][:, q * TPC:(q + 1) * TPC]
    return yT_full, outs


def build_fused(n_exp=NE):
    P = Prog()
    IN, OUT, SCR = "ExternalInput", "ExternalOutput", "Internal"
    L = DEPTH
    d = {}
    c_in = P.dram("c", [1, D], F32, IN)
    aw = P.dram("ada_w", [L, D, 6 * 1024], F32, IN)
    ab = P.dram("ada_b", [L, 6 * 1024], F32, IN)
    sel = P.dram("sel", [128, 12], F32, IN)
    x_b = P.dram("x_b", [SEQ, D], F32, IN)
    x_c = P.dram("x_c", [CT_TOK, D], F32, IN)
    x_own = P.dram("x_own", [TPC, D], F32, IN)
    wc = P.dram("wc", [L, D, WC], F32, IN)
    wa = P.dram("wa", [L, D, 2048], F32, IN)
    gmix = P.dram("g_mix", [L, D], F32, IN)
    cwT = P.dram("cwT", [L, CONV_CH, 31], F32, IN)
    cvec = P.dram("cvec", [L, 3, CONV_CH], F32, IN)
    halo = P.dram("halo", [128, 1], F32, IN)
    hv = P.dram("hv", [L, 4, 128], F32, IN)
    fb3 = P.dram("fb3", [L, 3, 1], F32, IN)
    dnp = P.dram("dnp", [L, 2, 4], F32, IN)
    dcw = P.dram("dcw", [L, 9 * 128, 4], F32, IN)
    ident = P.dram("ident", [128, 128], F32, IN)
    ut = P.dram("ut", [128, 128], F32, IN)
    cmask = P.dram("cmask", [128, 4, 512], BF16, IN)
    wout = P.dram("w_out", [L, D, D], F32, IN)
    gffn = P.dram("g_ffn", [L, D], F32, IN)
    fing = P.dram("final_g", [D], F32, IN)
    wr = P.dram("w_router", [D, NE], F32, IN)
    rb = P.dram("router_bias", [NE], F32, IN)
    wg = P.dram("w_gate", [L, NE, D, DE], F32, IN)
    wu = P.dram("w_up", [L, NE, D, DE], F32, IN)
    wd = P.dram("w_down", [L, NE, DE, D], F32, IN)
    outn = P.dram("outn", [TPC, D], F32, OUT)
    mod_sh = P.dram("mod_sh", [L * 6, 1024], F32, SCR)
    mod_all = P.dram("mod_all", [4 * L * 6, 1024], F32, SCR)
    modm = P.dram("modm", [2, D], F32, SCR)
    modv = P.dram("modv", [4, D], F32, SCR)
    ycomb = P.dram("ycomb", [768, SEQ], BF16, SCR)
    ygat = P.dram("ygat", [6, 4 * 128, SEQ], BF16, SCR)
    yaT = P.dram("yaT", [CONV_CH, 1024], BF16, SCR)
    yT = P.dram("yT", [D, TPC], BF16, SCR)
    x2 = P.dram("x2", [TPC, D], F32, SCR)
    xg = P.dram("xg", [16, 256, D], F32, SCR)
    x_c1 = P.dram("x_c1", [CT_TOK, D], F32, SCR)
    scrA = dict(hTd=P.dram("hTd", [128, KC, SEQ], BF16, SCR), hTc=P.dram("hTc", [128, KC, CT_TOK], BF16, SCR),
                projT=P.dram("projT", [NFM, 128, SEQ], F32, SCR), ffd=P.dram("ffd", [3, SEQ], F32, SCR),
                Fd=P.dram("Fd", [3, SEQ], F32, SCR), projM=P.dram("projM", [SEQ, TMW], F32, SCR),
                aT=P.dram("aT", [16, 128, CT_TOK], F32, SCR))
    scrB = dict(x1=P.dram("x1", [TPC, D], F32, SCR), acc=P.dram("acc", [TPC, D], F32, SCR),
                hTdB=P.dram("hT_scr", [128, KC, TPC], BF16, SCR),
                gates_d=P.dram("gates_scr", [128, NTT, NE], F32, SCR))
    G8 = [list(range(8))]
    G4 = [[0, 1, 2, 3], [4, 5, 6, 7]]

    def lv(t, l, name):
        return Tile(t.t[l], f"{name}{l}")

    def pv(parent, ap):
        v = Tile(ap, parent.trk.name)
        v.trk = parent.trk
        return v

    MQ = 6 * 1024
    P.begin_phase()
    cT = P.sb([128, KC, 1], F32, "cT")
    P.dma(cT[:, :, 0], c_in.t[0].rearrange("(k p) -> p k", p=128), reads=[c_in], writes=[cT], semtrk=cT,
          allow_slow_non_contiguous=True)
    cA = P.sb([128, KC, 1], F32, "cA")
    P.act(cA[:], cT[:], AF.Silu, [cT], [cA])
    wb = [P.sb([128, KC, 512], F32, f"w{i}") for i in range(2)]
    pss = [P.ps([128, 512], F32, f"ps{i}") for i in range(2)]
    res = P.sb([1, MQ], F32, "res")
    bias = P.sb([1, MQ], F32, "bias")
    it = 0
    for l in range(L):
        P.dma(bias[:], ab.t[l:l + 1, :], reads=[ab], writes=[bias], semtrk=bias)
        for ct in range(MQ // 512):
            w = wb[it % 2]
            ps = pss[it % 2]
            src = aw.t[l].rearrange("(k p) n -> p k n", p=128)[:, :, ct * 512:(ct + 1) * 512]
            half = KC // 2
            P.dma_group([(w[:, :half, :], src[:, :half, :]), (w[:, half:, :], src[:, half:, :])],
                        reads=[aw], writes=[w], semtrk=w, q="sync")
            for k in range(KC):
                P.mm(ps[:1, :], cA[:, k, :], w[:, k, :], k == 0, k == KC - 1, [cA, w], [ps])
            P.tt(res[:, ct * 512:(ct + 1) * 512], ps[:1, :], bias[:, ct * 512:(ct + 1) * 512], ALU.add,
                 [ps, bias], [res])
            it += 1
        P.dma(mod_sh.t[l * 6:(l + 1) * 6, :].rearrange("(o s) j -> o (s j)", o=1), res[:], reads=[res],
              writes=[mod_sh], semtrk=res)
    P.coll("AllGather", mod_all, mod_sh, G4)
    P.end_phase()
    mav = mod_all.t.rearrange("(r l s) j -> l s r j", r=4, l=L, s=6)

    for l in range(L):
        P.begin_phase()
        selt = P.sb([128, 12], F32, "selt")
        P.dma(selt[:], sel.t, reads=[sel], writes=[selt], semtrk=selt)
        for si in range(6):
            dst = (modm.t[si] if si < 2 else modv.t[si - 2]).rearrange("(r j) -> r j", j=1024)
            P.dma(dst, mav[l, si], reads=[mod_all], writes=[modm if si < 2 else modv], semtrk=Trk("d2d"))
        if l > 0:
            for c in range(16):
                P.coll("AllGather", pv(xg, xg.t[c]), pv(x2, x2.t[c * 64:(c + 1) * 64, :]), G4)
            P.dma(x_c1.t[128:, :], x2.t, reads=[x2], writes=[x_c1.sub("own")], semtrk=Trk("d2d"))
            h0 = P.sb([128, D], F32, "h0")
            cands = [P.sb([128, D], F32, f"cand{i}") for i in range(2)]
            for r in range(3):
                cd = cands[r % 2]
                P.dma_group([(cd[0:64, :], xg.t[14, r * 64:(r + 1) * 64, :]), (cd[64:128, :], xg.t[15, r * 64:(r + 1) * 64, :])],
                            reads=[xg], writes=[cd], semtrk=cd)
                if r == 0:
                    P.ts(h0[:], cd[:], selt[:, 6:7], None, ALU.mult, None, [cd, selt], [h0])
                else:
                    P.stt(h0[:], cd[:], selt[:, 6 + r:7 + r], h0[:], ALU.mult, ALU.add, [cd, selt, h0], [h0])
            P.dma(x_c1.t[0:128, :], h0[:], reads=[h0], writes=[x_c1.sub("halo")], semtrk=h0)
        P.end_phase()

        def xg_rows(tt):
            r, t8 = tt // 8, tt % 8
            return [(0, 64, xg.t[2 * t8, r * 64:(r + 1) * 64, :]), (64, 128, xg.t[2 * t8 + 1, r * 64:(r + 1) * 64, :])]

        ctxA = dict(P=P, x_b_rows=None if l == 0 else xg_rows, x_b=x_b if l == 0 else xg, x_c=x_c if l == 0 else x_c1, wc=lv(wc, l, "wc"),
                    wa=lv(wa, l, "wa"), modm=modm, gmix=lv(gmix, l, "gmix"), cwT=lv(cwT, l, "cwT"),
                    cvec=lv(cvec, l, "cvec"), halo=halo, hv=lv(hv, l, "hv"), fb3=lv(fb3, l, "fb3"),
                    dnp=lv(dnp, l, "dnp"), dcw=lv(dcw, l, "dcw"), ident=ident, ut=ut, cmask=cmask,
                    ybT=Tile(ycomb.t[0:384], "ybT"), ycT=Tile(ycomb.t[384:768], "ycT"), yaT=yaT, **scrA)
        build_A(ctx=ctxA)

        P.begin_phase()
        for p in range(6):
            P.coll("AllGather", pv(ygat, ygat.t[p]), pv(ycomb, ycomb.t[p * 128:(p + 1) * 128, :]), G4)
        P.dma(yT.t[0:CONV_CH, :], yaT.t, reads=[yaT], writes=[yT.sub("a")], semtrk=Trk("d2d"))
        selt = P.sb([128, 12], F32, "selt")
        P.dma(selt[:], sel.t, reads=[sel], writes=[selt], semtrk=selt)
        cnds = [P.sb([128, 4, TPC], BF16, f"cnd{i}") for i in range(3)]
        accs = [P.sb([128, TPC], BF16, f"yacc{i}") for i in range(3)]
        for ch in range(24):
            hd_ = ch % 12
            r, hh = hd_ // 3, hd_ % 3
            piece = hh if ch < 12 else 3 + hh
            cd = cnds[ch % 3]
            ac = accs[ch % 3]
            P.dma(cd[:], ygat.t[piece, r * 128:(r + 1) * 128, :].rearrange("p (q t) -> p q t", q=4), reads=[ygat],
                  writes=[cd], semtrk=cd)
            P.ts(ac[:], cd[:, 0, :], selt[:, 0:1], None, ALU.mult, None, [cd, selt], [ac])
            for qq in range(1, 4):
                P.stt(ac[:], cd[:, qq, :], selt[:, qq:qq + 1], ac[:], ALU.mult, ALU.add, [cd, selt, ac], [ac])
            P.dma(yT.t[CONV_CH + ch * 128:CONV_CH + (ch + 1) * 128, :], ac[:], reads=[ac], writes=[yT.sub(ch)],
                  semtrk=ac)
        P.end_phase()

        last = l == L - 1
        ctxB = dict(P=P, x=x_own if l == 0 else x2, yT=yT, wout=lv(wout, l, "wout"), modv=modv,
                    gffn=lv(gffn, l, "gffn"), fing=fing, wr=wr, rb=rb, wg=lv(wg, l, "wg"), wu=lv(wu, l, "wu"),
                    wd=lv(wd, l, "wd"), ident=ident, out=None if last else x2, outn=outn if last else None, **scrB)
        build_B(n_exp, ctx=ctxB)
    return P.finish()


def fused_inputs(inp):
    ident, ut, cmask = host_consts()
    x3 = inp["x"]
    L = DEPTH
    in_maps = []
    for i in range(8):
        b, q = i // 4, i % 4
        hs = [3 * q + hh for hh in range(3)]
        mcols = np.concatenate([np.arange(s * D + q * 1024, s * D + (q + 1) * 1024) for s in range(6)])
        cols = []
        for base in (O_FQ, O_FK, O_FG, O_DQ, O_DK, O_DV):
            for hh in hs:
                cols.append(np.arange(base + hh * 128, base + (hh + 1) * 128))
        cols.append(np.array([O_FF + hh for hh in hs]))
        for base in (O_FV, O_DZ):
            for hh in hs:
                cols.append(np.arange(base + hh * 128, base + (hh + 1) * 128))
        cols.append(np.array([O_DB + hh for hh in hs]))
        cols.append(np.array([O_DA + hh for hh in hs]))
        cols = np.concatenate(cols)
        xc = np.zeros((CT_TOK, D), np.float32)
        if q == 0:
            xc[128:] = x3[b, :1024]
        else:
            xc[:] = x3[b, q * 1024 - 128:(q + 1) * 1024]
        selv = np.zeros((12,), np.float32)
        selv[q] = 1.0
        selv[4 + b] = 1.0
        if q > 0:
            selv[6 + q - 1] = 1.0
        dcw_l, dnp_l = [], []
        for l in range(L):
            rows = []
            for comp in range(3):
                for hh in hs:
                    rows.append(inp["dn_conv_w"][l][:, comp * DN_W + hh * 128: comp * DN_W + (hh + 1) * 128].T)
            dcw_l.append(np.concatenate(rows, axis=0))
            dn = np.zeros((2, 4), np.float32)
            dn[0, :3] = inp["dn_a_log"][l][hs]
            dn[1, :3] = inp["dn_dt_bias"][l][hs]
            dnp_l.append(dn)
        in_maps.append({
            "c": np.ascontiguousarray(inp["c"][b:b + 1]),
            "ada_w": np.ascontiguousarray(inp["ada_w"][:, :, mcols]),
            "ada_b": np.ascontiguousarray(inp["ada_b"][:, mcols]),
            "sel": np.ascontiguousarray(np.broadcast_to(selv, (128, 12))),
            "x_b": np.ascontiguousarray(x3[b]), "x_c": xc,
            "x_own": np.ascontiguousarray(x3[b, q * TPC:(q + 1) * TPC]),
            "wc": np.ascontiguousarray(inp["w_in"][:, :, cols]),
            "wa": np.ascontiguousarray(inp["w_in"][:, :, :2048]),
            "g_mix": inp["norm_mix_g"],
            "cwT": np.ascontiguousarray(inp["conv_w"].transpose(0, 2, 1)),
            "cvec": np.ascontiguousarray(np.stack([inp["conv_b"], inp["conv_ln_g"], inp["conv_ln_b"]], axis=1)),
            "halo": np.full((128, 1), 0.0 if q == 0 else 1.0, np.float32),
            "hv": np.ascontiguousarray(np.stack([inp["fox_qn_g"], inp["fox_kn_g"], inp["fox_on_g"],
                                                 inp["dn_on_g"]], axis=1)),
            "fb3": np.ascontiguousarray(inp["fox_f_bias"][:, hs].reshape(L, 3, 1)),
            "dnp": np.stack(dnp_l), "dcw": np.ascontiguousarray(np.stack(dcw_l)),
            "ident": ident, "ut": ut, "cmask": cmask,
            "w_out": inp["w_out"], "g_ffn": inp["norm_ffn_g"], "final_g": inp["final_g"],
            "w_router": inp["w_router"], "router_bias": inp["router_bias"],
            "w_gate": inp["w_gate_e"], "w_up": inp["w_up_e"], "w_down": inp["w_down_e"],
        })
    return in_maps


def kernel(**inp):
    inp = {k: np.ascontiguousarray(np.asarray(v), dtype=np.float32) for k, v in inp.items()}
    nc = build_fused()
    in_maps = fused_inputs(inp)
    res = run_bass_kernel_spmd(nc, in_maps, core_ids=list(range(8)))
    out = np.concatenate([r["outn"] for r in res.results], axis=0)
    return out.reshape(NB, SEQ, D)
```
